# Optimizing a Trainium2 kernel written in Bass

```python
import math
import jax, jax.numpy as jnp
from jax import lax
import numpy as np

D_MODEL = 1024
BATCH = 8
SEQ = 2048
DEPTH = 4

GRID_W = 64
CTX_LEN = 256
N_MIXERS = 3
Q_BLOCK = 128
ROPE_THETA = 10000.0
LN_EPS = 1e-5
RMS_EPS = 1e-6
NEG_INF = -1e30

A_HEADS = 8
A_KV_HEADS = 2
A_HEAD_DIM = 128
B_HEADS = 16
B_KV_HEADS = 4
B_HEAD_DIM = 64
WINDOW = 128
C_HEADS = 8
C_HEAD_DIM = 64
N_GROUPS = 4
EXPERTS_PER_GROUP = 8
EXPERT_FF = 512
TOP_K = 2

N_A = (DEPTH + 2) // 3
N_B = (DEPTH + 1) // 3
N_C = DEPTH // 3

DEEPNORM_ALPHA = (2.0 * DEPTH) ** 0.25
DEEPNORM_BETA = (8.0 * DEPTH) ** -0.25

kernel_name = 'hybrid_diffusion_backbone'


def layer_norm(x, g, b):
    xf = x.astype(jnp.float32)
    mu = jnp.mean(xf, axis=-1, keepdims=True)
    var = jnp.mean(jnp.square(xf - mu), axis=-1, keepdims=True)
    y = (xf - mu) * lax.rsqrt(var + LN_EPS)
    return (y * g.astype(jnp.float32) + b.astype(jnp.float32)).astype(x.dtype)


def rms_norm(x, g):
    xf = x.astype(jnp.float32)
    y = xf * lax.rsqrt(jnp.mean(jnp.square(xf), axis=-1, keepdims=True) + RMS_EPS)
    return (y * g.astype(jnp.float32)).astype(x.dtype)


def axial_rope(n_tokens, head_dim):
    rows = n_tokens // GRID_W
    row = jnp.broadcast_to(jnp.arange(rows, dtype=jnp.float32)[:, None], (rows, GRID_W)).reshape(-1)
    col = jnp.broadcast_to(jnp.arange(GRID_W, dtype=jnp.float32)[None, :], (rows, GRID_W)).reshape(-1)
    n_freq = head_dim // 4
    inv = ROPE_THETA ** (-jnp.arange(n_freq, dtype=jnp.float32) / n_freq)
    ang = jnp.stack([row[:, None] * inv, col[:, None] * inv], axis=1)
    return jnp.cos(ang), jnp.sin(ang)


def apply_rope(x, cos, sin):
    nf = x.shape[-1] // 4
    xs = x.reshape(x.shape[:-1] + (2, 2, nf))
    x1, x2 = xs[..., 0, :], xs[..., 1, :]
    c, s = cos.astype(x.dtype), sin.astype(x.dtype)
    out = jnp.stack([x1 * c - x2 * s, x1 * s + x2 * c], axis=-2)
    return out.reshape(x.shape)


def to_blocks(x, axis):
    n = x.shape[axis] // Q_BLOCK
    x = x.reshape(x.shape[:axis] + (n, Q_BLOCK) + x.shape[axis + 1:])
    return jnp.moveaxis(x, axis, 0)


def from_blocks(x, axis):
    x = jnp.moveaxis(x, 0, axis)
    return x.reshape(x.shape[:axis] + (-1,) + x.shape[axis + 2:])


def _qkv_gqa(h, w_qkv, n_heads, n_kv, head_dim):
    b, t, _ = h.shape
    g = n_heads // n_kv
    q, k, v = jnp.split(h @ w_qkv, [n_heads * head_dim, (n_heads + n_kv) * head_dim], axis=-1)
    q = q.reshape(b, t, n_kv, g, head_dim).transpose(0, 2, 3, 1, 4)
    k = k.reshape(b, t, n_kv, head_dim).transpose(0, 2, 1, 3)
    v = v.reshape(b, t, n_kv, head_dim).transpose(0, 2, 1, 3)
    return q, k, v


def _merge_gqa(o):
    b, hkv, g, t, d = o.shape
    return o.transpose(0, 3, 1, 2, 4).reshape(b, t, hkv * g * d)


def _gqa_attend(q, k, v, scale):
    s = jnp.einsum('bhgqd,bhkd->bhgqk', q, k).astype(jnp.float32) * scale
    p = jax.nn.softmax(s, axis=-1).astype(v.dtype)
    return jnp.einsum('bhgqk,bhkd->bhgqd', p, v)


def mixer_a(h_lat, h_ctx, w_qkv, q_gain, k_gain, w_o, need_ctx):
    cos, sin = axial_rope(h_lat.shape[1], A_HEAD_DIM)
    scale = A_HEAD_DIM ** -0.5
    q, k, v = _qkv_gqa(h_lat, w_qkv, A_HEADS, A_KV_HEADS, A_HEAD_DIM)
    q = apply_rope(rms_norm(q, q_gain), cos, sin)
    k = apply_rope(rms_norm(k, k_gain), cos, sin)
    qc, kc, vc = _qkv_gqa(h_ctx, w_qkv, A_HEADS, A_KV_HEADS, A_HEAD_DIM)
    qc, kc = rms_norm(qc, q_gain), rms_norm(kc, k_gain)
    k_all = jnp.concatenate([k, kc], axis=2)
    v_all = jnp.concatenate([v, vc], axis=2)
    o = lax.map(lambda qb: _gqa_attend(qb, k_all, v_all, scale), to_blocks(q, 3))
    o_lat = _merge_gqa(from_blocks(o, 3)) @ w_o
    o_ctx = _merge_gqa(_gqa_attend(qc, kc, vc, scale)) @ w_o if need_ctx else None
    return o_lat, o_ctx


def mixer_b(h_lat, h_ctx, w_qkv, sink, w_o, need_ctx):
    s_len = h_lat.shape[1]
    cos, sin = axial_rope(s_len, B_HEAD_DIM)
    scale = B_HEAD_DIM ** -0.5
    g = B_HEADS // B_KV_HEADS
    q, k, v = _qkv_gqa(h_lat, w_qkv, B_HEADS, B_KV_HEADS, B_HEAD_DIM)
    q, k = apply_rope(q, cos, sin), apply_rope(k, cos, sin)
    qc, kc, vc = _qkv_gqa(h_ctx, w_qkv, B_HEADS, B_KV_HEADS, B_HEAD_DIM)
    n_ctx = kc.shape[2]
    span = Q_BLOCK + 2 * WINDOW
    pad = ((0, 0), (0, 0), (WINDOW, WINDOW), (0, 0))
    k_pad, v_pad = jnp.pad(k, pad), jnp.pad(v, pad)
    sink_l = sink.astype(jnp.float32).reshape(1, B_KV_HEADS, g, 1, 1)
    offs_q = jnp.arange(Q_BLOCK)
    offs_k = jnp.arange(span)
    ctx_valid = jnp.ones((Q_BLOCK, n_ctx), dtype=bool)

    def with_sink(s):
        return jnp.concatenate([s, jnp.broadcast_to(sink_l, s.shape[:-1] + (1,))], axis=-1)

    def block(args):
        j, qb = args
        start = j * Q_BLOCK
        kb = lax.dynamic_slice_in_dim(k_pad, start, span, axis=2)
        vb = lax.dynamic_slice_in_dim(v_pad, start, span, axis=2)
        qpos = start + offs_q
        kpos = start - WINDOW + offs_k
        valid = (jnp.abs(qpos[:, None] - kpos[None, :]) <= WINDOW) & ((kpos >= 0) & (kpos < s_len))[None, :]
        valid = jnp.concatenate([valid, ctx_valid], axis=1)
        kk = jnp.concatenate([kb, kc], axis=2)
        vv = jnp.concatenate([vb, vc], axis=2)
        s = jnp.einsum('bhgqd,bhkd->bhgqk', qb, kk).astype(jnp.float32) * scale
        s = jnp.where(valid, s, NEG_INF)
        p = jax.nn.softmax(with_sink(s), axis=-1)[..., :-1].astype(vv.dtype)
        return jnp.einsum('bhgqk,bhkd->bhgqd', p, vv)

    o = lax.map(block, (jnp.arange(s_len // Q_BLOCK), to_blocks(q, 3)))
    o_lat = _merge_gqa(from_blocks(o, 3)) @ w_o
    o_ctx = None
    if need_ctx:
        s = jnp.einsum('bhgqd,bhkd->bhgqk', qc, kc).astype(jnp.float32) * scale
        p = jax.nn.softmax(with_sink(s), axis=-1)[..., :-1].astype(vc.dtype)
        o_ctx = _merge_gqa(jnp.einsum('bhgqk,bhkd->bhgqd', p, vc)) @ w_o
    return o_lat, o_ctx


def _qkv_diff(h, w_qkv):
    b, t, _ = h.shape
    q, k, v = jnp.split(h @ w_qkv, 3, axis=-1)
    q = q.reshape(b, t, C_HEADS, 2, C_HEAD_DIM).transpose(0, 2, 3, 1, 4)
    k = k.reshape(b, t, C_HEADS, 2, C_HEAD_DIM).transpose(0, 2, 3, 1, 4)
    v = v.reshape(b, t, C_HEADS, 2 * C_HEAD_DIM).transpose(0, 2, 1, 3)
    return q, k, v


def _diff_attend(q, k, v, lam, scale):
    s = jnp.einsum('bhiqd,bhikd->bhiqk', q, k).astype(jnp.float32) * scale
    p = jax.nn.softmax(s, axis=-1)
    a = (p[:, :, 0] - lam * p[:, :, 1]).astype(v.dtype)
    return jnp.einsum('bhqk,bhkd->bhqd', a, v)


def _merge_diff(o, subln_gain, lam_init):
    o = rms_norm(o, subln_gain) * (1.0 - lam_init)
    b, h, t, d = o.shape
    return o.transpose(0, 2, 1, 3).reshape(b, t, h * d)


def mixer_c(h_lat, h_ctx, w_qkv, lam_q1, lam_k1, lam_q2, lam_k2, subln_gain, w_o, lam_init, need_ctx):
    cos, sin = axial_rope(h_lat.shape[1], C_HEAD_DIM)
    scale = C_HEAD_DIM ** -0.5
    lam = (jnp.exp(jnp.sum(lam_q1.astype(jnp.float32) * lam_k1.astype(jnp.float32)))
           - jnp.exp(jnp.sum(lam_q2.astype(jnp.float32) * lam_k2.astype(jnp.float32))) + lam_init)
    q, k, v = _qkv_diff(h_lat, w_qkv)
    q, k = apply_rope(q, cos, sin), apply_rope(k, cos, sin)
    qc, kc, vc = _qkv_diff(h_ctx, w_qkv)
    k_all = jnp.concatenate([k, kc], axis=3)
    v_all = jnp.concatenate([v, vc], axis=2)
    o = lax.map(lambda qb: _diff_attend(qb, k_all, v_all, lam, scale), to_blocks(q, 3))
    o_lat = _merge_diff(from_blocks(o, 2), subln_gain, lam_init) @ w_o
    o_ctx = _merge_diff(_diff_attend(qc, kc, vc, lam, scale), subln_gain, lam_init) @ w_o if need_ctx else None
    return o_lat, o_ctx


def hier_moe(h, w_group, b_group, w_expert, b_expert, w_gate, w_up, w_down):
    shp = h.shape
    t = h.reshape(-1, shp[-1])
    g_logits = (t @ w_group + b_group).astype(jnp.float32)
    g_sel = jnp.argmax(g_logits, axis=-1)
    g_prob = jnp.take_along_axis(jax.nn.softmax(g_logits, axis=-1), g_sel[:, None], axis=-1)
    e_logits = (t @ w_expert + b_expert).astype(jnp.float32).reshape(-1, N_GROUPS, EXPERTS_PER_GROUP)
    e_in_group = jnp.take_along_axis(e_logits, g_sel[:, None, None], axis=1)[:, 0]
    top_v, top_i = lax.top_k(e_in_group, TOP_K)
    top_w = jax.nn.softmax(top_v, axis=-1) * g_prob
    e_gate = jnp.sum(jax.nn.one_hot(top_i, EXPERTS_PER_GROUP, dtype=jnp.float32) * top_w[..., None], axis=1)
    combine = (jax.nn.one_hot(g_sel, N_GROUPS, dtype=jnp.float32)[:, :, None] * e_gate[:, None, :]).astype(t.dtype)
    y = jnp.zeros_like(t)
    for g in range(N_GROUPS):
        a = jnp.einsum('nd,edf->nef', t, w_gate[g])
        u = jnp.einsum('nd,edf->nef', t, w_up[g])
        hid = jax.nn.silu(a) * u * combine[:, g, :, None]
        y = y + jnp.einsum('nef,efd->nd', hid, w_down[g])
    return y.reshape(shp)


def setup_inputs(seed: int = 0) -> dict:
    key = jax.random.key(seed)
    ks = iter(jax.random.split(key, 32))
    D = D_MODEL

    def nrm(shape, scale):
        return jax.random.normal(next(ks), shape, jnp.float32) * scale

    a_qkv = (A_HEADS + 2 * A_KV_HEADS) * A_HEAD_DIM
    b_qkv = (B_HEADS + 2 * B_KV_HEADS) * B_HEAD_DIM
    c_qkv = 3 * 2 * C_HEADS * C_HEAD_DIM
    n_exp = N_GROUPS * EXPERTS_PER_GROUP
    return {
        'x': nrm((BATCH, SEQ, D), 1.0),
        'c': nrm((BATCH, D), 1.0),
        'ctx': nrm((BATCH, CTX_LEN, D), 1.0),
        'c_ctx': nrm((D,), 1.0),
        'w_mod': nrm((DEPTH, D, 6 * D), 0.5 * D ** -0.5),
        'b_mod': nrm((DEPTH, 6 * D), 0.02),
        'ln_gain': 1.0 + nrm((DEPTH, 2, D), 0.02),
        'ln_bias': nrm((DEPTH, 2, D), 0.02),
        'a_w_qkv': nrm((N_A, D, a_qkv), D ** -0.5),
        'a_q_gain': 1.0 + nrm((N_A, A_HEAD_DIM), 0.02),
        'a_k_gain': 1.0 + nrm((N_A, A_HEAD_DIM), 0.02),
        'a_w_o': nrm((N_A, A_HEADS * A_HEAD_DIM, D), DEEPNORM_BETA * (A_HEADS * A_HEAD_DIM) ** -0.5),
        'b_w_qkv': nrm((N_B, D, b_qkv), D ** -0.5),
        'b_sink': nrm((N_B, B_HEADS), 0.5),
        'b_w_o': nrm((N_B, B_HEADS * B_HEAD_DIM, D), DEEPNORM_BETA * (B_HEADS * B_HEAD_DIM) ** -0.5),
        'c_w_qkv': nrm((N_C, D, c_qkv), D ** -0.5),
        'c_lam_q1': nrm((N_C, C_HEAD_DIM), 0.1),
        'c_lam_k1': nrm((N_C, C_HEAD_DIM), 0.1),
        'c_lam_q2': nrm((N_C, C_HEAD_DIM), 0.1),
        'c_lam_k2': nrm((N_C, C_HEAD_DIM), 0.1),
        'c_subln_gain': 1.0 + nrm((N_C, 2 * C_HEAD_DIM), 0.02),
        'c_w_o': nrm((N_C, 2 * C_HEADS * C_HEAD_DIM, D), DEEPNORM_BETA * (2 * C_HEADS * C_HEAD_DIM) ** -0.5),
        'moe_w_group': nrm((DEPTH, D, N_GROUPS), D ** -0.5),
        'moe_b_group': nrm((DEPTH, N_GROUPS), 0.01),
        'moe_w_expert': nrm((DEPTH, D, n_exp), D ** -0.5),
        'moe_b_expert': nrm((DEPTH, n_exp), 0.01),
        'moe_w_gate': nrm((DEPTH, N_GROUPS, EXPERTS_PER_GROUP, D, EXPERT_FF), D ** -0.5),
        'moe_w_up': nrm((DEPTH, N_GROUPS, EXPERTS_PER_GROUP, D, EXPERT_FF), D ** -0.5),
        'moe_w_down': nrm((DEPTH, N_GROUPS, EXPERTS_PER_GROUP, EXPERT_FF, D), DEEPNORM_BETA * EXPERT_FF ** -0.5),
    }


def reference(x, c, ctx, c_ctx, w_mod, b_mod, ln_gain, ln_bias,
              a_w_qkv, a_q_gain, a_k_gain, a_w_o,
              b_w_qkv, b_sink, b_w_o,
              c_w_qkv, c_lam_q1, c_lam_k1, c_lam_q2, c_lam_k2, c_subln_gain, c_w_o,
              moe_w_group, moe_b_group, moe_w_expert, moe_b_expert, moe_w_gate, moe_w_up, moe_w_down):
    n_ctx = ctx.shape[1]
    xl, xc = x, ctx
    silu_c = jax.nn.silu(c)
    silu_cc = jax.nn.silu(c_ctx)
    for i in range(DEPTH):
        need_ctx = i < DEPTH - 1
        m_lat = (silu_c @ w_mod[i] + b_mod[i])[:, None, :]
        m_ctx = (silu_cc @ w_mod[i] + b_mod[i])[None, None, :]
        sh1, sc1, g1, sh2, sc2, g2 = jnp.split(m_lat, 6, axis=-1)
        csh1, csc1, cg1, csh2, csc2, cg2 = jnp.split(m_ctx, 6, axis=-1)
        h_lat = xl * (1.0 + sc1) + sh1
        h_ctx = xc * (1.0 + csc1) + csh1
        kind, j = i % N_MIXERS, i // N_MIXERS
        if kind == 0:
            o_lat, o_ctx = mixer_a(h_lat, h_ctx, a_w_qkv[j], a_q_gain[j], a_k_gain[j], a_w_o[j], need_ctx)
        elif kind == 1:
            o_lat, o_ctx = mixer_b(h_lat, h_ctx, b_w_qkv[j], b_sink[j], b_w_o[j], need_ctx)
        else:
            lam_init = 0.8 - 0.6 * math.exp(-0.3 * i)
            o_lat, o_ctx = mixer_c(h_lat, h_ctx, c_w_qkv[j], c_lam_q1[j], c_lam_k1[j], c_lam_q2[j], c_lam_k2[j],
                                   c_subln_gain[j], c_w_o[j], lam_init, need_ctx)
        xl = layer_norm(DEEPNORM_ALPHA * xl + g1 * o_lat, ln_gain[i, 0], ln_bias[i, 0])
        moe_args = (moe_w_group[i], moe_b_group[i], moe_w_expert[i], moe_b_expert[i],
                    moe_w_gate[i], moe_w_up[i], moe_w_down[i])
        if need_ctx:
            xc = layer_norm(DEEPNORM_ALPHA * xc + cg1 * o_ctx, ln_gain[i, 0], ln_bias[i, 0])
            h = jnp.concatenate([xc * (1.0 + csc2) + csh2, xl * (1.0 + sc2) + sh2], axis=1)
            f = hier_moe(h, *moe_args)
            f_ctx, f_lat = f[:, :n_ctx], f[:, n_ctx:]
            xc = layer_norm(DEEPNORM_ALPHA * xc + cg2 * f_ctx, ln_gain[i, 1], ln_bias[i, 1])
        else:
            f_lat = hier_moe(xl * (1.0 + sc2) + sh2, *moe_args)
        xl = layer_norm(DEEPNORM_ALPHA * xl + g2 * f_lat, ln_gain[i, 1], ln_bias[i, 1])
    return xl
```

```python
import contextlib
import math
import numpy as np
import concourse.bass as bass
import concourse.mybir as mybir
from concourse.bass_utils import run_bass_kernel_spmd

F32 = mybir.dt.float32
BF16 = mybir.dt.bfloat16
AF = mybir.ActivationFunctionType
ALU = mybir.AluOpType
AX = mybir.AxisListType

D = 1024
SEQ = 2048
NCTX = 256
NT = SEQ + NCTX
NTILE = NT // 128
DEPTH = 4
ALPHA = (2.0 * DEPTH) ** 0.25
LN_EPS = 1e-5
RMS_EPS = 1e-6
THETA = 10000.0
ST = [(0, 512), (512, 512), (1024, 512), (1536, 512), (2048, 256)]

DMA_RING = 8


class _Op:
    __slots__ = ("eng", "fn", "deps", "dma", "sig", "dsem", "dval", "idx")

    def __init__(self, eng, fn, deps, dma, idx):
        self.eng = eng
        self.fn = fn
        self.deps = deps
        self.dma = dma
        self.sig = 0
        self.dsem = None
        self.dval = 0
        self.idx = idx


class Prog:
    ENGS = ("pe", "act", "dve", "pool", "sp")

    def __init__(self, nc):
        self.nc = nc
        self.ops = []
        self.last_w = {}
        self.readers = {}
        self.dma_hist = {e: [] for e in self.ENGS}
        self.last_on = {}
        self.bar = {}

    def add(self, eng, fn, r=(), w=(), dma=False):
        deps = set()
        for b in r:
            lw = self.last_w.get(b)
            if lw is not None:
                deps.add(lw)
        for b in w:
            lw = self.last_w.get(b)
            if lw is not None:
                deps.add(lw)
            for x in self.readers.get(b, ()):
                deps.add(x)
        if eng in self.bar:
            deps |= self.bar.pop(eng)
        idx = len(self.ops)
        op = _Op(eng, fn, deps, dma, idx)
        if dma:
            h = self.dma_hist[eng]
            if len(h) >= DMA_RING:
                deps.add(h[-DMA_RING])
            h.append(idx)
        else:
            self.last_on[eng] = idx
        for b in r:
            self.readers.setdefault(b, []).append(idx)
        for b in w:
            self.last_w[b] = idx
            self.readers[b] = []
        self.ops.append(op)
        return idx

    def barrier(self):
        s = set(self.last_on.values())
        for e in self.ENGS:
            s |= set(self.dma_hist[e][-DMA_RING:])
        for e in self.ENGS:
            self.bar[e] = set(s) | self.bar.get(e, set())

    def pe(self, fn, r=(), w=()):
        return self.add("pe", fn, r, w)

    def act(self, fn, r=(), w=()):
        return self.add("act", fn, r, w)

    def dve(self, fn, r=(), w=()):
        return self.add("dve", fn, r, w)

    def pool(self, fn, r=(), w=()):
        return self.add("pool", fn, r, w)

    def dma(self, eng, fn, r=(), w=()):
        return self.add(eng, fn, r, w, dma=True)

    def emit(self):
        nc = self.nc
        ops = self.ops
        needed = set()
        for op in ops:
            for d in op.deps:
                dop = ops[d]
                if dop.dma:
                    continue
                if dop.eng == "pe" and op.eng == "pe" and not op.dma:
                    continue
                needed.add(d)
        cnt = {e: 0 for e in self.ENGS}
        dcount = {e: 0 for e in self.ENGS}
        last_compute = {}
        for op in ops:
            if op.dma:
                n = dcount[op.eng]
                dcount[op.eng] += 1
                op.dsem = (op.eng, n % DMA_RING)
                op.dval = 16 * (n // DMA_RING + 1)
            else:
                last_compute[op.eng] = op.idx
        for e, i in last_compute.items():
            needed.add(i)
        for op in ops:
            if not op.dma and op.idx in needed:
                cnt[op.eng] += 1
                op.sig = cnt[op.eng]
        with contextlib.ExitStack() as es:
            csem = {e: es.enter_context(nc.semaphore("s_" + e)) for e in self.ENGS}
            dsem = {}
            for e in self.ENGS:
                for k in range(min(DMA_RING, dcount[e])):
                    dsem[(e, k)] = es.enter_context(nc.semaphore("d_%s_%d" % (e, k)))
            block = es.enter_context(nc.Block())
            by_eng = {e: [op for op in ops if op.eng == e] for e in self.ENGS}

            def run(engname, eng):
                waited = {}
                for op in by_eng[engname]:
                    for d in sorted(op.deps):
                        dop = ops[d]
                        if dop.dma:
                            sem, val, key = dsem[dop.dsem], dop.dval, ("d",) + dop.dsem
                        else:
                            if dop.eng == "pe" and engname == "pe" and not op.dma:
                                continue
                            sem, val, key = csem[dop.eng], dop.sig, ("c", dop.eng)
                        if waited.get(key, 0) >= val:
                            continue
                        waited[key] = val
                        eng.wait_ge(sem, val)
                    ins = op.fn(eng)
                    if op.dma:
                        ins.then_inc(dsem[op.dsem], 16)
                    elif op.sig:
                        ins.then_inc(csem[engname], 1)
                if engname == "sp":
                    for e in self.ENGS:
                        if cnt[e] > 0 and e != "sp":
                            eng.wait_ge(csem[e], cnt[e])
                    for e in self.ENGS:
                        n = dcount[e]
                        for k in range(min(DMA_RING, n)):
                            last = ((n - 1 - k) // DMA_RING) * DMA_RING + k
                            eng.wait_ge(dsem[(e, k)], 16 * (last // DMA_RING + 1))

            @block.tensor
            def _(eng):
                run("pe", eng)

            @block.scalar
            def _(eng):
                run("act", eng)

            @block.vector
            def _(eng):
                run("dve", eng)

            @block.gpsimd
            def _(eng):
                run("pool", eng)

            @block.sync
            def _(eng):
                run("sp", eng)


def _consts():
    ident = np.eye(128, dtype=np.float32)

    def rmat(hd):
        nf = hd // 4
        R = np.zeros((128, 128), np.float32)
        for b in range(0, 128, hd):
            for ax in range(2):
                o = b + ax * 2 * nf
                for f in range(nf):
                    R[o + f, o + nf + f] = -1.0
                    R[o + nf + f, o + f] = 1.0
        return np.ascontiguousarray(R.T)

    def tab(hd):
        nf = hd // 4
        t = np.arange(SEQ)
        row = (t // 64).astype(np.float32)
        col = (t % 64).astype(np.float32)
        inv = (np.float32(THETA) ** (-np.arange(nf, dtype=np.float32) / np.float32(nf))).astype(np.float32)
        out = np.zeros((2, 128, SEQ), np.float32)
        for p in range(128):
            d = p % hd
            ax = d // (2 * nf)
            f = d % nf
            pos = row if ax == 0 else col
            ang = (pos * inv[f]).astype(np.float32)
            out[0, p] = np.cos(ang)
            out[1, p] = np.sin(ang)
        return out

    mask = np.zeros((128, 6, 512), np.float32)
    k = np.arange(128)[:, None]
    q = np.arange(512)[None, :]
    for ri in range(6):
        rel = ri - 1
        mask[:, ri, :] = (np.abs(q - rel * 128 - k) <= 128).astype(np.float32)
    return {"k_ident": ident, "k_rt128": rmat(128), "k_rt64": rmat(64), "k_tabA": tab(128), "k_tabB": tab(64),
            "k_mask": mask}


INPUT_SHAPES = {
    "x": [SEQ, D], "c": [D], "ctx": [NCTX, D], "c_ctx": [D],
    "w_mod": [4, D, 6 * D], "b_mod": [4, 6 * D], "ln_gain": [4, 2, D], "ln_bias": [4, 2, D],
    "a_w_qkv": [2, D, 1536], "a_q_gain": [2, 128], "a_k_gain": [2, 128], "a_w_o": [2, D, D],
    "b_w_qkv": [1, D, 1536], "b_sink": [1, 16], "b_w_o": [1, D, D],
    "c_w_qkv": [1, D, 3072], "c_lam_q1": [1, 64], "c_lam_k1": [1, 64], "c_lam_q2": [1, 64], "c_lam_k2": [1, 64],
    "c_subln_gain": [1, 128], "c_w_o": [1, D, D],
    "moe_w_group": [4, D, 4], "moe_b_group": [4, 4], "moe_w_expert": [4, D, 32], "moe_b_expert": [4, 32],
    "moe_w_gate": [4, 4, 8, D, 512], "moe_w_up": [4, 4, 8, D, 512], "moe_w_down": [4, 4, 8, 512, D],
    "k_ident": [128, 128], "k_rt128": [128, 128], "k_rt64": [128, 128], "k_tabA": [2, 128, SEQ],
    "k_tabB": [2, 128, SEQ], "k_mask": [128, 6, 512],
}


class _Stop(Exception):
    pass


def build_nc(n_layers=DEPTH, n_experts=32, dbg=False, stop=None):
    nc = bass.Bass("TRN2", target_bir_lowering=False)
    I = {k: nc.dram_tensor(k, v, F32, kind="ExternalInput").ap() for k, v in INPUT_SHAPES.items()}
    out = nc.dram_tensor("out", [SEQ, D], F32, kind="ExternalOutput").ap()
    dbg_x = nc.dram_tensor("dbg_x", [NT, D], F32, kind="ExternalOutput").ap() if dbg else None
    xs = nc.dram_tensor("xs_scr", [NT, D], F32, kind="Internal").ap()
    md = nc.dram_tensor("md_scr", [4, 2, 6 * D], F32, kind="Internal").ap()

    base = [16512]

    def alloc(name, shape, dt, off=None):
        nbytes = int(np.prod(shape[1:])) * (4 if dt == F32 else 2)
        if off is None:
            off = base[0]
            base[0] += (nbytes + 63) // 64 * 64
        return nc.alloc_sbuf_tensor_at(name, shape, dt, offset=off)

    identF = alloc("identF", [128, 128], F32)
    ones_bf = alloc("ones_bf", [128, 128], BF16)
    rt128 = alloc("rt128", [128, 128], BF16)
    rt64 = alloc("rt64", [128, 128], BF16)
    cT = alloc("cT", [128, 8, 2], F32)
    sT = alloc("sT", [128, 8, 2], F32)
    modT = alloc("modT", [128, 8, 8], F32)
    small = alloc("small", [128, 64], F32)
    lamv = alloc("lamv", [128, 4, 64], F32)
    esb = alloc("esb", [128, 16], F32)
    lnp = alloc("lnp", [128, 2, D], F32)
    gb = alloc("gb", [128, 2, D], F32)
    wr = alloc("wr", [128, 8, 36], F32)
    brb = alloc("brb", [128, 36], F32)
    comb = alloc("comb", [128, NTILE, 32], F32)
    rscr = alloc("rscr", [128, 208], F32)
    onesF = alloc("onesF", [1, 128], F32)
    W0 = base[0]
    KB = 1024
    acc = alloc("acc", [128, NTILE, D], F32, W0)
    kT = alloc("kT", [128, 4, NT], BF16, W0)
    Vb = alloc("Vb", [128, NTILE, 512], BF16, W0 + 18 * KB)
    maskb = alloc("maskb", [128, 6, 512], BF16, W0 + 36 * KB)
    cosT = alloc("cosT", [128, SEQ], F32, W0 + 42 * KB)
    sinT = alloc("sinT", [128, SEQ], F32, W0 + 50 * KB)
    wv = alloc("wv", [128, 8, 512], BF16, W0 + 58 * KB)
    hT = alloc("hT", [128, 8, NT], BF16, W0 + 72 * KB)
    qT = alloc("qT", [128, 8, NT], BF16, W0 + 108 * KB)
    gu = alloc("gu", [128, 2, 8, 512], BF16, W0 + 108 * KB)
    dr = alloc("dr", [128, 4, D], BF16, W0 + 124 * KB)
    dl = alloc("dl", [128, 4, D], BF16, W0 + 132 * KB)
    M0 = W0 + 144 * KB
    wring = [alloc("wring%d" % i, [128, 8, 128], BF16, M0 + 2 * KB * i) for i in range(4)]
    w_o = alloc("w_o", [128, 8, D], BF16, M0)
    hid = alloc("hid", [128, 4, NT], BF16, M0)
    S = [alloc("S%d" % i, [128, 512], F32, M0 + 18 * KB + 2 * KB * i) for i in range(5)]
    hTf = alloc("hTf", [128, 8, 128], F32, M0 + 18 * KB + 2 * KB * 3)
    qz = alloc("qz", [128, 2, 512], BF16, M0 + 18 * KB + 2 * KB * 4)
    Bs = [alloc("B%d" % i, [128, 512], BF16, M0 + 28 * KB + KB * i) for i in range(4)]
    xt = [alloc("xt%d" % i, [128, D], F32, M0 + 32 * KB + 4 * KB * i) for i in range(2)]
    end = M0 + 40 * KB
    assert end <= 229344, end
    wm = [alloc("wm%d" % i, [128, 8, 512], F32, W0 + 16 * KB * i) for i in range(2)]
    m_sb = alloc("m_sb", [2, 6 * D], F32, W0 + 32 * KB)
    bm = alloc("bm", [2, 6 * D], F32, W0 + 56 * KB)

    ps = [nc.alloc_psum_tensor("ps%d" % i, [128, 512], F32) for i in range(8)]
    PS = ["ps%d" % i for i in range(8)]

    P = Prog(nc)
    dcnt = [0]

    def dump(ap, row0, n, is_bf=True):
        k = dcnt[0] % 2
        dcnt[0] += 1
        P.barrier()
        P.act(lambda e: e.activation(out=xt[k][:, 0:n], in_=ap, func=AF.Identity), w=["xt%d" % k])
        P.dma("sp", lambda e: e.dma_start(out=dbg_x[row0:row0 + 128, 0:n], in_=xt[k][:, 0:n]), r=["xt%d" % k], w=["dbgd"])
        P.barrier()

    def checkpoint(name, fn):
        if stop == name:
            P.barrier()
            fn()
            raise _Stop()

    P.dma("sp", lambda e: e.dma_start(out=identF[:], in_=I["k_ident"]), w=["identF"])
    P.dma("pool", lambda e: e.dma_start(out=rt128[:], in_=I["k_rt128"]), w=["rt128"])
    P.dma("pool", lambda e: e.dma_start(out=rt64[:], in_=I["k_rt64"]), w=["rt64"])
    P.dve(lambda e: e.memset(ones_bf[:], 1.0), w=["ones_bf"])
    P.dve(lambda e: e.memset(onesF[:], 1.0), w=["onesF"])
    P.dma("sp", lambda e: e.dma_start(out=cT[:, :, 0], in_=I["c"].rearrange("(c p) -> p c", p=128)), w=["cT"])
    P.dma("sp", lambda e: e.dma_start(out=cT[:, :, 1], in_=I["c_ctx"].rearrange("(c p) -> p c", p=128)), w=["cT"])
    P.act(lambda e: e.activation(out=sT[:], in_=cT[:], func=AF.Silu), r=["cT"], w=["sT"])

    cnt = 0
    for i in range(n_layers):
        for r_ in range(2):
            P.dma("sp", lambda e, i=i, r_=r_: e.dma_start(out=bm[r_:r_ + 1, :], in_=I["b_mod"][i:i + 1, :]), w=["bm"])
        for j in range(12):
            s = cnt % 2
            cnt += 1
            P.dma("sp", lambda e, i=i, j=j, s=s: e.dma_start(
                out=wm[s][:], in_=I["w_mod"][i][:, j * 512:(j + 1) * 512].rearrange("(c p) n -> p c n", p=128)),
                w=["wm%d" % s])
            pb = ps[j % 2]
            for kc in range(8):
                P.pe(lambda e, s=s, kc=kc, pb=pb: e.matmul(pb[0:2, :], lhsT=sT[:, kc, :], rhs=wm[s][:, kc, :],
                                                           start=(kc == 0), stop=(kc == 7)),
                     r=["sT", "wm%d" % s], w=[PS[j % 2]])
            P.dve(lambda e, j=j, pb=pb: e.tensor_tensor(out=m_sb[0:2, j * 512:(j + 1) * 512], in0=pb[0:2, :],
                                                        in1=bm[0:2, j * 512:(j + 1) * 512], op=ALU.add),
                  r=[PS[j % 2], "bm"], w=["m_sb"])
        P.dma("sp", lambda e, i=i: e.dma_start(out=md[i], in_=m_sb[0:2, :]), r=["m_sb"], w=["md%d" % i])
    P.barrier()

    def _d_pro():
        P.dma("sp", lambda e: e.dma_start(out=dbg_x[0:12, :], in_=md[0].rearrange("r (a d) -> (r a) d", d=D)), r=["md0"], w=["dbgd"])
    try:
        checkpoint("pro", _d_pro)
    except _Stop:
        with nc.allow_non_contiguous_dma(reason="dbg"):
            P.emit()
        return nc

    def bcast_row(dst_fn, n, src_row, dst_names):
        P.dma("sp", lambda e: e.dma_start(out=xt[1][0:1, 0:n], in_=src_row.rearrange("(o n) -> o n", o=1)), w=["xt1"])
        for c0 in range(0, n, 512):
            w_ = min(512, n - c0)
            P.pe(lambda e, c0=c0, w_=w_: e.matmul(ps[7][:, 0:w_], lhsT=onesF[0:1, :], rhs=xt[1][0:1, c0:c0 + w_], start=True, stop=True),
                 r=["xt1", "onesF"], w=[PS[7]])
            P.act(lambda e, c0=c0, w_=w_: e.activation(out=dst_fn(c0, w_), in_=ps[7][:, 0:w_], func=AF.Identity), r=[PS[7]], w=dst_names)

    def load_mod(i):
        for slot, (v, row) in enumerate([(0, 0), (1, 0), (0, 1), (1, 1), (3, 0), (4, 0), (3, 1), (4, 1)]):
            P.dma("sp", lambda e, slot=slot, v=v, row=row: e.dma_start(
                out=modT[:, slot, :], in_=md[i][row, v * D:(v + 1) * D].rearrange("(c p) -> p c", p=128)),
                r=["md%d" % i], w=["modT"])
        for slot in (1, 3, 5, 7):
            P.dve(lambda e, slot=slot: e.tensor_scalar(out=modT[:, slot, :], in0=modT[:, slot, :], scalar1=1.0,
                                                       scalar2=None, op0=ALU.add), r=["modT"], w=["modT"])

    def load_gate(i, v):
        for row in range(2):
            bcast_row(lambda c0, w_, row=row: gb[:, row, c0:c0 + w_], D, md[i][row, v * D:(v + 1) * D], ["gb"])

    def load_ln(i, j):
        bcast_row(lambda c0, w_: lnp[:, 0, c0:c0 + w_], D, I["ln_gain"][i, j, :], ["lnp"])
        bcast_row(lambda c0, w_: lnp[:, 1, c0:c0 + w_], D, I["ln_bias"][i, j, :], ["lnp"])

    def layer_norm_tile(src, dst, nm_src, nm_dst):
        st6 = rscr[:, 0:12].rearrange("p (a b) -> p a b", a=2)
        mv = rscr[:, 12:14]
        for h in range(2):
            P.dve(lambda e, h=h: e.bn_stats(out=st6[:, h, :], in_=src[:, h * 512:(h + 1) * 512]), r=[nm_src], w=["st6"])
        P.dve(lambda e: e.bn_aggr(out=mv, in_=rscr[:, 0:12]), r=["st6"], w=["mv"])
        P.act(lambda e: e.activation(out=rscr[:, 14:15], in_=rscr[:, 13:14], func=AF.Ln, bias=small[:, 0:1], scale=1.0),
              r=["mv", "small"], w=["sd"])
        P.act(lambda e: e.activation(out=rscr[:, 15:16], in_=rscr[:, 14:15], func=AF.Exp, scale=-0.5), r=["sd"], w=["rstd"])
        P.dve(lambda e: e.tensor_scalar(out=rscr[:, 16:17], in0=rscr[:, 12:13], scalar1=rscr[:, 15:16], scalar2=-1.0,
                                        op0=ALU.mult, op1=ALU.mult), r=["mv", "rstd"], w=["nb"])
        P.act(lambda e: e.activation(out=dst, in_=src, func=AF.Identity, scale=rscr[:, 15:16], bias=rscr[:, 16:17]),
              r=[nm_src, "rstd", "nb"], w=[nm_dst])
        P.pool(lambda e: e.tensor_tensor(out=dst, in0=dst, in1=lnp[:, 0, :], op=ALU.mult), r=[nm_dst, "lnp"], w=[nm_dst])
        P.pool(lambda e: e.tensor_tensor(out=dst, in0=dst, in1=lnp[:, 1, :], op=ALU.add), r=[nm_dst, "lnp"], w=[nm_dst])

    def build_hT_tile(src, nm_src, t, sc_slot, sh_slot, with_f32, pb2):
        for c in range(8):
            pb = ps[pb2 + c // 4]
            P.pe(lambda e, c=c, pb=pb: e.transpose(out=pb[:, (c % 4) * 128:(c % 4 + 1) * 128],
                                                   in_=src[:, c * 128:(c + 1) * 128], identity=identF[:]),
                 r=[nm_src, "identF"], w=[PS[pb2 + c // 4]])
        for c in range(8):
            pb = ps[pb2 + c // 4]
            pin = pb[:, (c % 4) * 128:(c % 4 + 1) * 128]
            dst = hT[:, c, t * 128:(t + 1) * 128]
            if with_f32:
                P.dve(lambda e, pin=pin, c=c: e.tensor_scalar(out=hTf[:, c, :], in0=pin, scalar1=modT[:, sc_slot, c:c + 1],
                                                              scalar2=modT[:, sh_slot, c:c + 1], op0=ALU.mult, op1=ALU.add),
                      r=[PS[pb2 + c // 4], "modT"], w=["hTf_%d" % c])
                P.act(lambda e, dst=dst, c=c: e.activation(out=dst, in_=hTf[:, c, :], func=AF.Identity),
                      r=["hTf_%d" % c], w=["hT_%d" % t])
            elif c % 2 == 0:
                P.act(lambda e, pin=pin, dst=dst, c=c: e.activation(out=dst, in_=pin, func=AF.Identity,
                                                                    scale=modT[:, sc_slot, c:c + 1],
                                                                    bias=modT[:, sh_slot, c:c + 1]),
                      r=[PS[pb2 + c // 4], "modT"], w=["hT_%d" % t])
            else:
                P.dve(lambda e, pin=pin, dst=dst, c=c: e.tensor_scalar(out=dst, in0=pin, scalar1=modT[:, sc_slot, c:c + 1],
                                                                       scalar2=modT[:, sh_slot, c:c + 1], op0=ALU.mult,
                                                                       op1=ALU.add),
                      r=[PS[pb2 + c // 4], "modT"], w=["hT_%d" % t])

    def hT_names(t0, n):
        return ["hT_%d" % t for t in range(t0 // 128, (t0 + n) // 128)]

    P.dve(lambda e: e.memset(small[:, 0:1], LN_EPS), w=["small"])
    P.dve(lambda e: e.memset(small[:, 1:2], RMS_EPS), w=["small"])

    def layer(i):
        kind, jj = i % 3, i // 3
        last = (i == DEPTH - 1)
        need_ctx = not last
        n_tok_tiles = NTILE if need_ctx else 16
        wqkv = [I["a_w_qkv"], I["b_w_qkv"], I["c_w_qkv"]][kind][jj]
        wo_d = [I["a_w_o"], I["b_w_o"], I["c_w_o"]][kind][jj]
        hd = 128 if kind == 0 else 64
        scale = hd ** -0.5
        rt = rt128 if kind == 0 else rt64
        rtn = "rt128" if kind == 0 else "rt64"
        tabd = I["k_tabA"] if kind == 0 else I["k_tabB"]

        load_mod(i)
        if i > 0:
            P.barrier()
        def p1a_a(t):
            s = t % 2
            if i == 0:
                src_d = I["x"][t * 128:(t + 1) * 128, :] if t < 16 else I["ctx"][(t - 16) * 128:(t - 15) * 128, :]
                P.dma("sp", lambda e, s=s, src_d=src_d: e.dma_start(out=xt[s][:], in_=src_d), w=["xt%d" % s])
            else:
                layer_norm_tile(acc[:, t, :], xt[s][:], "acc_%d" % t, "xt%d" % s)
                P.dma("sp", lambda e, s=s, t=t: e.dma_start(out=xs[t * 128:(t + 1) * 128, :], in_=xt[s][:]),
                      r=["xt%d" % s], w=["xs_%d" % t])

        def p1a_b(t):
            s = t % 2
            lat = t < 16
            build_hT_tile(xt[s][:], "xt%d" % s, t, 1 if lat else 3, 0 if lat else 2, False, 2 * (t % 2))

        for t in range(NTILE + 1):
            if t < NTILE:
                p1a_a(t)
            if t >= 1:
                p1a_b(t - 1)
        P.barrier()

        def _d_p1a():
            for c in range(8):
                dump(hT[:, c, 0:1024], c * 128, 1024)
            for c in range(8):
                dump(hT[:, c, 2048:2304], 1024 + c * 128, 256)
        checkpoint("p1a_%d" % i, _d_p1a)

        P.dma("sp", lambda e: e.dma_start(out=cosT[:], in_=tabd[0]), w=["cosT"])
        P.dma("sp", lambda e: e.dma_start(out=sinT[:], in_=tabd[1]), w=["sinT"])
        if kind == 0:
            P.dma("sp", lambda e: e.dma_start(out=small[:, 2:3], in_=I["a_q_gain"][jj].rearrange("(p o) -> p o", o=1)), w=["small"])
            P.dma("sp", lambda e: e.dma_start(out=small[:, 3:4], in_=I["a_k_gain"][jj].rearrange("(p o) -> p o", o=1)), w=["small"])
        elif kind == 1:
            bcast_row(lambda c0, w_: esb[:, c0:c0 + w_], 16, I["b_sink"][0, :], ["esb"])
            P.act(lambda e: e.activation(out=esb[:], in_=esb[:], func=AF.Exp), r=["esb"], w=["esb"])
            P.dma("pool", lambda e: e.dma_start(out=maskb[:], in_=I["k_mask"]), w=["maskb"])
        else:
            lam_init = 0.8 - 0.6 * math.exp(-0.3 * i)
            for q_, nm in enumerate(["c_lam_q1", "c_lam_k1", "c_lam_q2", "c_lam_k2"]):
                bcast_row(lambda c0, w_, q_=q_: lamv[:, q_, c0:c0 + w_], 64, I[nm][0, :], ["lamv"])
            P.dve(lambda e: e.tensor_tensor(out=lamv[:, 0, :], in0=lamv[:, 0, :], in1=lamv[:, 1, :], op=ALU.mult), r=["lamv"], w=["lamv"])
            P.dve(lambda e: e.tensor_tensor(out=lamv[:, 2, :], in0=lamv[:, 2, :], in1=lamv[:, 3, :], op=ALU.mult), r=["lamv"], w=["lamv"])
            P.dve(lambda e: e.tensor_reduce(out=small[:, 4:5], in_=lamv[:, 0, :], axis=AX.X, op=ALU.add), r=["lamv"], w=["small"])
            P.dve(lambda e: e.tensor_reduce(out=small[:, 5:6], in_=lamv[:, 2, :], axis=AX.X, op=ALU.add), r=["lamv"], w=["small"])
            P.act(lambda e: e.activation(out=small[:, 4:6], in_=small[:, 4:6], func=AF.Exp), r=["small"], w=["small"])
            P.dve(lambda e: e.tensor_tensor(out=small[:, 6:7], in0=small[:, 5:6], in1=small[:, 4:5], op=ALU.subtract), r=["small"], w=["small"])
            P.dve(lambda e: e.tensor_scalar(out=small[:, 6:7], in0=small[:, 6:7], scalar1=-lam_init, scalar2=None, op0=ALU.add),
                  r=["small"], w=["small"])
            P.dma("sp", lambda e: e.dma_start(out=small[:, 7:8], in_=I["c_subln_gain"][0].rearrange("(p o) -> p o", o=1)), w=["small"])
            P.dve(lambda e: e.tensor_scalar(out=small[:, 7:8], in0=small[:, 7:8], scalar1=1.0 - lam_init, scalar2=None, op0=ALU.mult),
                  r=["small"], w=["small"])

        sidx = [0]
        bidx = [0]
        if kind != 0:
            P.pool(lambda e: e.memset(qz[64:128, 0, :], 0.0), w=["S4"])
            P.pool(lambda e: e.memset(qz[0:64, 1, :], 0.0), w=["S4"])

        def fill_qz(src_lo, src_hi, n, rnames):
            if src_lo is not None:
                P.dve(lambda e: e.tensor_copy(out=qz[0:64, 0, 0:n], in_=src_lo), r=rnames, w=["S4"])
            if src_hi is not None:
                P.dve(lambda e: e.tensor_copy(out=qz[64:128, 1, 0:n], in_=src_hi), r=rnames, w=["S4"])

        def nS():
            sidx[0] = (sidx[0] + 1) % (5 if kind == 0 else 4)
            return S[sidx[0]], "S%d" % sidx[0]

        def nB():
            bidx[0] = (bidx[0] + 1) % 4
            return Bs[bidx[0]], "B%d" % bidx[0]

        wcnt = [0]

        def load_wchunk(cols_list):
            s = wcnt[0] % 4
            wcnt[0] += 1
            for (d0, s0, n) in cols_list:
                P.dma("pool", lambda e, s=s, d0=d0, s0=s0, n=n: e.dma_start(
                    out=wring[s][:, :, d0:d0 + n], in_=wqkv[:, s0:s0 + n].rearrange("(c p) n -> p c n", p=128)),
                    w=["wring%d" % s])
            return wring[s], "wring%d" % s

        pcnt = [0]

        def proj_fm(wt, wnm, dstT, dst_c, dst_nm, mode):
            for (t0, n) in ST:
                if t0 >= SEQ and False:
                    continue
                is_ctx = t0 >= SEQ
                a = pcnt[0] % 2
                pcnt[0] += 1
                pA, nA = ps[a], PS[a]
                pB, nB_ = ps[2 + a], PS[2 + a]
                pC, nC = ps[4 + a], PS[4 + a]
                for kc in range(8):
                    P.pe(lambda e, kc=kc, pA=pA, t0=t0, n=n: e.matmul(pA[:, 0:n], lhsT=wt[:, kc, :], rhs=hT[:, kc, t0:t0 + n],
                                                                     start=(kc == 0), stop=(kc == 7)),
                         r=[wnm] + hT_names(t0, n), w=[nA])
                dst = dstT[:, dst_c, t0:t0 + n]
                dnm = "%s_%d_%d" % (dst_nm, dst_c, t0)
                if mode in ("A_q", "A_k"):
                    gcol = small[:, 2:3] if mode == "A_q" else small[:, 3:4]
                    sq, sqn = nB()
                    P.act(lambda e, sq=sq, pA=pA, n=n: e.activation(out=sq[:, 0:n], in_=pA[:, 0:n], func=AF.Square), r=[nA], w=[sqn])
                    P.pe(lambda e, sq=sq, pB=pB, n=n: e.matmul(pB[:, 0:n], lhsT=ones_bf[:], rhs=sq[:, 0:n], start=True, stop=True),
                         r=[sqn, "ones_bf"], w=[nB_])
                    qg, qgn = nB()
                    P.act(lambda e, qg=qg, pA=pA, n=n, gcol=gcol: e.activation(out=qg[:, 0:n], in_=pA[:, 0:n], func=AF.Identity, scale=gcol),
                          r=[nA, "small"], w=[qgn])
                    rs, rsn = nS()
                    P.act(lambda e, rs=rs, pB=pB, n=n: e.activation(out=rs[:, 0:n], in_=pB[:, 0:n], func=AF.Ln, scale=1.0 / 128,
                                                                   bias=small[:, 1:2]), r=[nB_, "small"], w=[rsn])
                    P.act(lambda e, rs=rs, n=n: e.activation(out=rs[:, 0:n], in_=rs[:, 0:n], func=AF.Exp, scale=-0.5), r=[rsn], w=[rsn])
                    if not is_ctx:
                        P.pe(lambda e, qg=qg, pC=pC, n=n: e.matmul(pC[:, 0:n], lhsT=rt[:], rhs=qg[:, 0:n], start=True, stop=True),
                             r=[qgn, rtn], w=[nC])
                        t1, t1n = nS()
                        P.pool(lambda e, t1=t1, qg=qg, n=n, t0=t0: e.tensor_tensor(out=t1[:, 0:n], in0=qg[:, 0:n], in1=cosT[:, t0:t0 + n], op=ALU.mult),
                               r=[qgn, "cosT"], w=[t1n])
                        t2, t2n = nS()
                        P.dve(lambda e, t2=t2, pC=pC, n=n, t0=t0: e.tensor_tensor(out=t2[:, 0:n], in0=pC[:, 0:n], in1=sinT[:, t0:t0 + n], op=ALU.mult),
                              r=[nC, "sinT"], w=[t2n])
                        P.dve(lambda e, t1=t1, t2=t2, n=n: e.tensor_tensor(out=t1[:, 0:n], in0=t1[:, 0:n], in1=t2[:, 0:n], op=ALU.add),
                              r=[t1n, t2n], w=[t1n])
                        P.dve(lambda e, t1=t1, rs=rs, n=n, dst=dst: e.tensor_tensor(out=dst, in0=t1[:, 0:n], in1=rs[:, 0:n], op=ALU.mult),
                              r=[t1n, rsn], w=[dnm])
                    else:
                        P.dve(lambda e, qg=qg, rs=rs, n=n, dst=dst: e.tensor_tensor(out=dst, in0=qg[:, 0:n], in1=rs[:, 0:n], op=ALU.mult),
                              r=[qgn, rsn], w=[dnm])
                else:
                    if not is_ctx:
                        qb, qbn = nB()
                        P.act(lambda e, qb=qb, pA=pA, n=n: e.activation(out=qb[:, 0:n], in_=pA[:, 0:n], func=AF.Identity), r=[nA], w=[qbn])
                        P.pe(lambda e, qb=qb, pC=pC, n=n: e.matmul(pC[:, 0:n], lhsT=rt[:], rhs=qb[:, 0:n], start=True, stop=True),
                             r=[qbn, rtn], w=[nC])
                        t1, t1n = nS()
                        P.dve(lambda e, t1=t1, pA=pA, n=n, t0=t0: e.tensor_tensor(out=t1[:, 0:n], in0=pA[:, 0:n], in1=cosT[:, t0:t0 + n], op=ALU.mult),
                              r=[nA, "cosT", qbn], w=[t1n])
                        t2, t2n = nS()
                        P.dve(lambda e, t2=t2, pC=pC, n=n, t0=t0: e.tensor_tensor(out=t2[:, 0:n], in0=pC[:, 0:n], in1=sinT[:, t0:t0 + n], op=ALU.mult),
                              r=[nC, "sinT"], w=[t2n])
                        P.pool(lambda e, t1=t1, t2=t2, n=n, dst=dst: e.tensor_tensor(out=dst, in0=t1[:, 0:n], in1=t2[:, 0:n], op=ALU.add),
                               r=[t1n, t2n], w=[dnm])
                    else:
                        P.act(lambda e, pA=pA, n=n, dst=dst: e.activation(out=dst, in_=pA[:, 0:n], func=AF.Identity), r=[nA], w=[dnm])

        def names_fm(nm, c, t0, n):
            res = []
            for (a, b) in ST:
                if a < t0 + n and t0 < a + b:
                    res.append("%s_%d_%d" % (nm, c, a))
            return res

        for g in range(2):
            if kind == 0:
                for j in range(4):
                    wt, wnm = load_wchunk([(0, (4 * g + j) * 128, 128)])
                    proj_fm(wt, wnm, qT, 4 * g + j, "qT", "A_q")
                wt, wnm = load_wchunk([(0, 1024 + g * 128, 128)])
                proj_fm(wt, wnm, kT, 0, "kT", "A_k")
                vcols, vsrc = 128, 1280 + g * 128
            elif kind == 1:
                for j in range(4):
                    wt, wnm = load_wchunk([(0, (4 * g + j) * 128, 128)])
                    proj_fm(wt, wnm, qT, 4 * g + j, "qT", "rope")
                for j in range(2):
                    c0 = 1024 + (2 * g + j) * 64
                    wt, wnm = load_wchunk([(0, c0, 64), (64, c0, 64)])
                    proj_fm(wt, wnm, kT, j, "kT", "rope")
                vcols, vsrc = 128, 1280 + (2 * g) * 64
            else:
                for j in range(4):
                    wt, wnm = load_wchunk([(0, (4 * g + j) * 128, 128)])
                    proj_fm(wt, wnm, qT, 4 * g + j, "qT", "rope")
                for j in range(4):
                    wt, wnm = load_wchunk([(0, 1024 + (4 * g + j) * 128, 128)])
                    proj_fm(wt, wnm, kT, j, "kT", "rope")
                vcols, vsrc = 512, 2048 + 4 * g * 128
            P.dma("pool", lambda e, vcols=vcols, vsrc=vsrc: e.dma_start(
                out=wv[:, :, 0:vcols], in_=wqkv[:, vsrc:vsrc + vcols].rearrange("(c p) n -> p c n", p=128)), w=["wv"])
            if kind == 1:
                P.pool(lambda e: e.memset(Vb[:], 1.0), w=["Vb_%d" % t for t in range(NTILE)])
            for t in range(NTILE):
                pv, pvn = ps[6 + t % 2], PS[6 + t % 2]
                for kc in range(8):
                    P.pe(lambda e, kc=kc, pv=pv, t=t, vcols=vcols: e.matmul(pv[:, 0:vcols], lhsT=hT[:, kc, t * 128:(t + 1) * 128],
                                                                             rhs=wv[:, kc, 0:vcols], start=(kc == 0), stop=(kc == 7)),
                         r=["wv", "hT_%d" % t], w=[pvn])
                if kind == 1:
                    P.act(lambda e, pv=pv, t=t: e.activation(out=Vb[:, t, 0:256].rearrange("p (h d) -> p h d", h=2)[:, :, 0:64],
                                                             in_=pv[:, 0:128].rearrange("p (h d) -> p h d", h=2), func=AF.Identity),
                          r=[pvn], w=["Vb_%d" % t])
                else:
                    P.act(lambda e, pv=pv, t=t, vcols=vcols: e.activation(out=Vb[:, t, 0:vcols], in_=pv[:, 0:vcols], func=AF.Identity),
                          r=[pvn], w=["Vb_%d" % t])

            def _d_p1b():
                for j in range(4):
                    dump(qT[:, 4 * g + j, 0:1024], j * 128, 1024)
                for j in range(4):
                    dump(kT[:, j, 0:1024], 512 + j * 128, 1024)
                for j in range(4):
                    dump(qT[:, 4 * g + j, 2048:2304], 1024 + j * 128, 256)
                for j in range(4):
                    dump(kT[:, j, 2048:2304], 1536 + j * 128, 256)
                dump(Vb[:, 0, :], 2048, 512)
                dump(Vb[:, 17, :], 2176, 512)
            checkpoint("p1b_%d_%d" % (i, g), _d_p1b)

            acnt = [0]

            def attend(q_aps, k_fn, v_fn, qnames, t0, n, kcs, out_fn, sink_h=None, kmask=None, krange=None):
                nm = len(q_aps)
                a = acnt[0] % 2
                acnt[0] += 1
                if nm == 1:
                    pS = [[ps[0], ps[1], ps[2], ps[3]]]
                    nSn = [[PS[0], PS[1], PS[2], PS[3]]]
                    pO, nO = [ps[4 + a]], [PS[4 + a]]
                    pZ, nZ = [ps[6 + a]], [PS[6 + a]]
                else:
                    pS = [[ps[0], ps[1]], [ps[2], ps[3]]]
                    nSn = [[PS[0], PS[1]], [PS[2], PS[3]]]
                    pO, nO = [ps[4], ps[5]], [PS[4], PS[5]]
                    pZ, nZ = [ps[6], ps[7]], [PS[6], PS[7]]
                steps = [(ki, kc, m) for ki, kc in enumerate(kcs) for m in range(nm)]
                pts = {}

                def qk(ki, kc, m):
                    c0, c1 = krange(kc) if krange is not None else (0, n)
                    pSb = pS[m][ki % len(pS[m])]
                    pSn = nSn[m][ki % len(pS[m])]
                    kap, knames = k_fn(m, kc)
                    P.pe(lambda e, pSb=pSb, kap=kap, m=m: e.matmul(pSb[:, c0:c1], lhsT=kap, rhs=q_aps[m][:, c0:c1], start=True, stop=True),
                         r=knames + qnames, w=[pSn])
                    pt, ptn = nB()
                    P.act(lambda e, pt=pt, pSb=pSb: e.activation(out=pt[:, c0:c1], in_=pSb[:, c0:c1], func=AF.Exp, scale=scale),
                          r=[pSn], w=[ptn])
                    if kmask is not None and kmask(kc) is not None:
                        ri = kmask(kc)
                        P.dve(lambda e, pt=pt, ri=ri: e.tensor_tensor(out=pt[:, c0:c1], in0=pt[:, c0:c1], in1=maskb[:, ri, c0:c1], op=ALU.mult),
                              r=[ptn, "maskb"], w=[ptn])
                    pts[(ki, m)] = (pt, ptn, c0, c1)

                def pv(ki, kc, m):
                    pt, ptn, c0, c1 = pts.pop((ki, m))
                    vap, vnames = v_fn(kc)
                    P.pe(lambda e, pt=pt, vap=vap, m=m, ki=ki: e.matmul(pO[m][:, c0:c1], lhsT=vap, rhs=pt[:, c0:c1],
                                                                      start=(ki == 0), stop=(ki == len(kcs) - 1)),
                         r=[ptn] + vnames, w=[nO[m]])
                    if sink_h is None:
                        P.pe(lambda e, pt=pt, m=m, ki=ki: e.matmul(pZ[m][:, c0:c1], lhsT=ones_bf[:], rhs=pt[:, c0:c1],
                                                                  start=(ki == 0), stop=(ki == len(kcs) - 1)),
                             r=[ptn, "ones_bf"], w=[nZ[m]])

                LA = 3
                for s_ in range(len(steps) + LA):
                    if s_ < len(steps):
                        qk(*steps[s_])
                    if s_ >= LA:
                        pv(*steps[s_ - LA])
                out_fn(pO, pZ, nO, nZ)

            lat_kcs = list(range(NTILE))
            ctx_kcs = [16, 17]
            if kind == 0:
                for j in range(4):
                    h = 4 * g + j
                    for (t0, n) in ST:
                        if t0 >= SEQ and not need_ctx:
                            continue
                        kcs = lat_kcs if t0 < SEQ else ctx_kcs

                        def out_fn(pO, pZ, nO, nZ, h=h, t0=t0, n=n):
                            rz, rzn = nS()
                            P.act(lambda e: e.activation(out=rz[:, 0:n], in_=pZ[0][:, 0:n], func=AF.Ln), r=[nZ[0]], w=[rzn])
                            P.act(lambda e: e.activation(out=rz[:, 0:n], in_=rz[:, 0:n], func=AF.Exp, scale=-1.0), r=[rzn], w=[rzn])
                            P.dve(lambda e: e.tensor_tensor(out=qT[:, h, t0:t0 + n], in0=pO[0][:, 0:n], in1=rz[:, 0:n], op=ALU.mult),
                                  r=[nO[0], rzn], w=["qT_%d_%d" % (h, t0)])

                        attend([qT[:, h, t0:t0 + n]],
                               lambda m, kc: (kT[:, 0, kc * 128:(kc + 1) * 128], names_fm("kT", 0, kc * 128, 128)),
                               lambda kc: (Vb[:, kc, 0:128], ["Vb_%d" % kc]),
                               ["qT_%d_%d" % (h, t0)], t0, n, kcs, out_fn)
            elif kind == 1:
                for hh in range(8):
                    h = 8 * g + hh
                    kvl = hh // 4
                    qc = h // 2
                    ph = 64 * (h % 2)
                    for ti, (t0, n) in enumerate(ST):
                        if t0 >= SEQ and not need_ctx:
                            continue
                        if t0 < SEQ:
                            kcs = ctx_kcs + [kc for kc in range(4 * ti - 1, 4 * ti + 5) if 0 <= kc < 16]
                            kmask = (lambda kc, ti=ti: (kc - 4 * ti + 1) if kc < 16 else None)

                            def krange(kc, ti=ti, n=n):
                                if kc >= 16:
                                    return (0, n)
                                rel = kc - 4 * ti
                                return (128 * max(0, rel - 1), 128 * (min(3, rel + 1) + 1))
                        else:
                            kcs = ctx_kcs
                            kmask = None
                            krange = None

                        def out_fn(pO, pZ, nO, nZ, h=h, qc=qc, ph=ph, t0=t0, n=n):
                            zs, zsn = nS()
                            P.act(lambda e: e.activation(out=zs[64:128, 0:n], in_=pO[0][64:128, 0:n], func=AF.Ln, bias=esb[64:128, h:h + 1], scale=1.0),
                                  r=[nO[0], "esb"], w=[zsn])
                            P.act(lambda e: e.activation(out=zs[64:128, 0:n], in_=zs[64:128, 0:n], func=AF.Exp, scale=-1.0), r=[zsn], w=[zsn])
                            P.dve(lambda e: e.tensor_tensor(out=qT[ph:ph + 64, qc, t0:t0 + n], in0=pO[0][0:64, 0:n], in1=zs[64:128, 0:n], op=ALU.mult),
                                  r=[nO[0], zsn], w=["qT_%d_%d" % (qc, t0)])

                        if ph == 0:
                            fill_qz(qT[0:64, qc, t0:t0 + n], None, n, ["qT_%d_%d" % (qc, t0)])
                        else:
                            fill_qz(None, qT[64:128, qc, t0:t0 + n], n, ["qT_%d_%d" % (qc, t0)])
                        attend([qz[:, h % 2, 0:n]],
                               lambda m, kc, kvl=kvl: (kT[:, kvl, kc * 128:(kc + 1) * 128], names_fm("kT", kvl, kc * 128, 128)),
                               lambda kc, kvl=kvl: (Vb[:, kc, kvl * 128:(kvl + 1) * 128], ["Vb_%d" % kc]),
                               ["S4"], t0, n, kcs, out_fn, sink_h=h, kmask=kmask, krange=krange)
            else:
                for j in range(4):
                    h = 4 * g + j
                    for (t0, n) in ST:
                        if t0 >= SEQ and not need_ctx:
                            continue
                        kcs = lat_kcs if t0 < SEQ else ctx_kcs

                        def out_fn(pO, pZ, nO, nZ, h=h, t0=t0, n=n):
                            S0, S1, S2, S3_, S4_ = S
                            P.dve(lambda e: e.tensor_copy(out=S0[:, 0:n], in_=pZ[0][:, 0:n]), r=[nZ[0]], w=["S0"])
                            P.dve(lambda e: e.tensor_copy(out=S1[:, 0:n], in_=pZ[1][:, 0:n]), r=[nZ[1]], w=["S1"])
                            P.dve(lambda e: e.tensor_copy(out=S2[:, 0:n], in_=pO[0][:, 0:n]), r=[nO[0]], w=["S2"])
                            P.dve(lambda e: e.tensor_copy(out=S3_[:, 0:n], in_=pO[1][:, 0:n]), r=[nO[1]], w=["S3"])
                            P.dve(lambda e: e.reciprocal(out=S0[:, 0:n], in_=S0[:, 0:n]), r=["S0"], w=["S0"])
                            P.dve(lambda e: e.reciprocal(out=S1[:, 0:n], in_=S1[:, 0:n]), r=["S1"], w=["S1"])
                            P.dve(lambda e: e.tensor_tensor(out=S2[:, 0:n], in0=S2[:, 0:n], in1=S0[:, 0:n], op=ALU.mult), r=["S2", "S0"], w=["S2"])
                            P.pool(lambda e: e.tensor_tensor(out=S3_[:, 0:n], in0=S3_[:, 0:n], in1=S1[:, 0:n], op=ALU.mult), r=["S3", "S1"], w=["S3"])
                            P.dve(lambda e: e.scalar_tensor_tensor(out=qT[:, h, t0:t0 + n], in0=S3_[:, 0:n], scalar=small[:, 6:7], in1=S2[:, 0:n],
                                                                   op0=ALU.mult, op1=ALU.add), r=["S2", "S3", "small"], w=["qT_%d_%d" % (h, t0)])

                        fill_qz(qT[0:64, h, t0:t0 + n], qT[64:128, h, t0:t0 + n], n, ["qT_%d_%d" % (h, t0)])
                        attend([qz[:, 0, 0:n], qz[:, 1, 0:n]],
                               lambda m, kc, j=j: (kT[:, j, kc * 128:(kc + 1) * 128], names_fm("kT", j, kc * 128, 128)),
                               lambda kc, j=j: (Vb[:, kc, j * 128:(j + 1) * 128], ["Vb_%d" % kc]),
                               ["S4"], t0, n, kcs, out_fn)
            if kind == 2:
                kk = 0
                for j in range(4):
                    h = 4 * g + j
                    for (t0, n) in ST:
                        if t0 >= SEQ and not need_ctx:
                            continue
                        pb, pbn = ps[kk % 4], PS[kk % 4]
                        kk += 1
                        sq, sqn = nB()
                        onm = "qT_%d_%d" % (h, t0)
                        P.act(lambda e, sq=sq, h=h, t0=t0, n=n: e.activation(out=sq[:, 0:n], in_=qT[:, h, t0:t0 + n], func=AF.Square), r=[onm], w=[sqn])
                        P.pe(lambda e, sq=sq, pb=pb, n=n: e.matmul(pb[:, 0:n], lhsT=ones_bf[:], rhs=sq[:, 0:n], start=True, stop=True),
                             r=[sqn, "ones_bf"], w=[pbn])
                        rs, rsn = nS()
                        P.act(lambda e, rs=rs, pb=pb, n=n: e.activation(out=rs[:, 0:n], in_=pb[:, 0:n], func=AF.Ln, scale=1.0 / 128, bias=small[:, 1:2]),
                              r=[pbn, "small"], w=[rsn])
                        P.act(lambda e, rs=rs, n=n: e.activation(out=rs[:, 0:n], in_=rs[:, 0:n], func=AF.Exp, scale=-0.5), r=[rsn], w=[rsn])
                        P.dve(lambda e, rs=rs, h=h, t0=t0, n=n: e.scalar_tensor_tensor(out=qT[:, h, t0:t0 + n], in0=qT[:, h, t0:t0 + n], scalar=small[:, 7:8],
                                                                                       in1=rs[:, 0:n], op0=ALU.mult, op1=ALU.mult),
                              r=[onm, rsn, "small"], w=[onm])
            P.barrier()

        def _d_p2():
            for c in range(8):
                dump(qT[:, c, 0:1024], c * 128, 1024)
            for c in range(8):
                dump(qT[:, c, 2048:2304], 1024 + c * 128, 256)
        checkpoint("p2_%d" % i, _d_p2)

        load_gate(i, 2)
        load_ln(i, 0)
        P.dma("pool", lambda e: e.dma_start(out=w_o[:], in_=wo_d.rearrange("(c p) n -> p c n", p=128)), w=["w_o"])
        P.dma("sp", lambda e: e.dma_start(out=wr[:, :, 0:4], in_=I["moe_w_group"][i].rearrange("(c p) n -> p c n", p=128)), w=["wr"])
        P.dma("sp", lambda e: e.dma_start(out=wr[:, :, 4:36], in_=I["moe_w_expert"][i].rearrange("(c p) n -> p c n", p=128)), w=["wr"])
        bcast_row(lambda c0, w_: brb[:, c0:c0 + w_], 4, I["moe_b_group"][i, :], ["brb"])
        bcast_row(lambda c0, w_: brb[:, 4 + c0:4 + c0 + w_], 32, I["moe_b_expert"][i, :], ["brb"])
        def _d_p3a():
            dump(gb[:, 0, :], 0, 1024, False)
            dump(gb[:, 1, :], 128, 1024, False)
            dump(lnp[:, 0, :], 256, 1024, False)
            dump(lnp[:, 1, :], 384, 1024, False)
            dump(brb[:], 512, 36, False)
            dump(w_o[:, 0, :], 640, 1024)
        checkpoint("p3a_%d" % i, _d_p3a)
        def p3_a(t):
            lat = t < 16
            s = t % 2
            if i == 0:
                src_d = I["x"][t * 128:(t + 1) * 128, :] if lat else I["ctx"][(t - 16) * 128:(t - 15) * 128, :]
            else:
                src_d = xs[t * 128:(t + 1) * 128, :]
            P.dma("sp", lambda e, s=s, src_d=src_d: e.dma_start(out=xt[s][:], in_=src_d), r=["xs_%d" % t], w=["xt%d" % s])
            qn = []
            for c in range(8):
                qn += names_fm("qT", c, t * 128, 128)
            for half in range(2):
                py, pyn = ps[half], PS[half]
                for c in range(8):
                    P.pe(lambda e, c=c, py=py, t=t, half=half: e.matmul(py[:], lhsT=qT[:, c, t * 128:(t + 1) * 128],
                                                                         rhs=w_o[:, c, half * 512:(half + 1) * 512],
                                                                         start=(c == 0), stop=(c == 7)),
                         r=["w_o"] + qn, w=[pyn])
                tb, tbn = S[half], "S%d" % half
                P.dve(lambda e, tb=tb, py=py, half=half, lat=lat: e.tensor_tensor(out=tb[:], in0=py[:], in1=gb[:, 0 if lat else 1, half * 512:(half + 1) * 512],
                                                                                   op=ALU.mult), r=[pyn, "gb"], w=[tbn])
                P.dve(lambda e, tb=tb, t=t, half=half, s=s: e.scalar_tensor_tensor(out=acc[:, t, half * 512:(half + 1) * 512],
                                                                                    in0=xt[s][:, half * 512:(half + 1) * 512], scalar=ALPHA,
                                                                                    in1=tb[:], op0=ALU.mult, op1=ALU.add),
                      r=[tbn, "xt%d" % s], w=["acc_%d" % t])

        def p3_ln(t):
            layer_norm_tile(acc[:, t, :], acc[:, t, :], "acc_%d" % t, "acc_%d" % t)

        def p3_b(t):
            lat = t < 16
            build_hT_tile(acc[:, t, :], "acc_%d" % t, t, 5 if lat else 7, 4 if lat else 6, True, 2 + 2 * (t % 2))
            pr, prn = ps[6 + t % 2], PS[6 + t % 2]
            for c in range(8):
                P.pe(lambda e, c=c, pr=pr: e.matmul(pr[:, 0:36], lhsT=hTf[:, c, :], rhs=wr[:, c, :], start=(c == 0), stop=(c == 7)),
                     r=["hTf_%d" % c, "wr"], w=[prn])
            P.act(lambda e, t=t: e.activation(out=acc[:, t, :], in_=acc[:, t, :], func=AF.Identity, scale=ALPHA),
                  r=["acc_%d" % t], w=["acc_%d" % t])
            L = rscr[:, 32:68]
            P.dve(lambda e, pr=pr: e.tensor_tensor(out=rscr[:, 32:68], in0=pr[:, 0:36], in1=brb[:], op=ALU.add), r=[prn, "brb"], w=["L"])
            E3 = rscr[:, 36:68].rearrange("p (g e) -> p g e", g=4)
            gmax, gsum, gsel = rscr[:, 70:71], rscr[:, 72:73], rscr[:, 76:80]
            ge, dlt = rscr[:, 80:84], rscr[:, 84:88]
            m1, m2, w1, w2 = rscr[:, 88:92], rscr[:, 92:96], rscr[:, 96:100], rscr[:, 100:104]
            eq1 = rscr[:, 104:136].rearrange("p (g e) -> p g e", g=4)
            eq2 = rscr[:, 136:168].rearrange("p (g e) -> p g e", g=4)
            E2 = rscr[:, 168:200].rearrange("p (g e) -> p g e", g=4)
            gw = rscr[:, 200:204]

            def bc(ap4):
                return ap4.unsqueeze(2).broadcast_to([128, 4, 8])

            rt_ = ["L", "rt_"]
            P.dve(lambda e: e.tensor_reduce(out=gmax, in_=rscr[:, 32:36], axis=AX.X, op=ALU.max), r=["L"], w=["rt_"])
            P.dve(lambda e: e.tensor_scalar(out=gsel, in0=rscr[:, 32:36], scalar1=gmax, scalar2=None, op0=ALU.is_equal), r=rt_, w=["rt_"])
            P.dve(lambda e: e.tensor_scalar(out=ge, in0=rscr[:, 32:36], scalar1=gmax, scalar2=None, op0=ALU.subtract), r=rt_, w=["rt_"])
            P.dve(lambda e: e.tensor_reduce(out=m1, in_=E3, axis=AX.X, op=ALU.max), r=rt_, w=["rt_"])
            P.dve(lambda e: e.tensor_tensor(out=eq1, in0=E3, in1=bc(m1), op=ALU.is_equal), r=rt_, w=["rt_"])
            P.dve(lambda e: e.scalar_tensor_tensor(out=E2, in0=eq1, scalar=-1e30, in1=E3, op0=ALU.mult, op1=ALU.add), r=rt_, w=["rt_"])
            P.dve(lambda e: e.tensor_reduce(out=m2, in_=E2, axis=AX.X, op=ALU.max), r=rt_, w=["rt_"])
            P.dve(lambda e: e.tensor_tensor(out=eq2, in0=E2, in1=bc(m2), op=ALU.is_equal), r=rt_, w=["rt_"])
            P.dve(lambda e: e.tensor_tensor(out=dlt, in0=m2, in1=m1, op=ALU.subtract), r=rt_, w=["rt_"])
            P.act(lambda e: e.activation(out=rscr[:, 80:88], in_=rscr[:, 80:88], func=AF.Exp), r=["rt_"], w=["rt_b"])
            P.dve(lambda e: e.tensor_reduce(out=gsum, in_=ge, axis=AX.X, op=ALU.add), r=["rt_b", "rt_"], w=["rt_"])
            P.dve(lambda e: e.reciprocal(out=gsum, in_=gsum), r=rt_, w=["rt_"])
            P.dve(lambda e: e.tensor_scalar(out=gw, in0=gsel, scalar1=gsum, scalar2=None, op0=ALU.mult), r=rt_, w=["rt_"])
            P.dve(lambda e: e.tensor_scalar(out=w1, in0=dlt, scalar1=1.0, scalar2=None, op0=ALU.add), r=["rt_b", "rt_"], w=["rt_"])
            P.dve(lambda e: e.reciprocal(out=w1, in_=w1), r=rt_, w=["rt_"])
            P.dve(lambda e: e.tensor_tensor(out=w2, in0=dlt, in1=w1, op=ALU.mult), r=["rt_", "rt_b"], w=["rt_"])
            P.dve(lambda e: e.tensor_tensor(out=w1, in0=w1, in1=gw, op=ALU.mult), r=rt_, w=["rt_"])
            P.dve(lambda e: e.tensor_tensor(out=w2, in0=w2, in1=gw, op=ALU.mult), r=rt_, w=["rt_"])
            P.dve(lambda e: e.tensor_tensor(out=eq1, in0=eq1, in1=bc(w1), op=ALU.mult), r=rt_, w=["rt_"])
            P.dve(lambda e: e.tensor_tensor(out=eq2, in0=eq2, in1=bc(w2), op=ALU.mult), r=rt_, w=["rt_"])
            P.dve(lambda e, t=t: e.tensor_tensor(out=comb[:, t, :].rearrange("p (g e) -> p g e", g=4), in0=eq1, in1=eq2, op=ALU.add),
                  r=rt_, w=["comb_%d" % t])

        for t in range(n_tok_tiles + 2):
            if t < n_tok_tiles:
                p3_a(t)
            if 1 <= t < n_tok_tiles + 1:
                p3_ln(t - 1)
            if t >= 2:
                p3_b(t - 2)
        P.barrier()

        def _d_p3():
            for t in range(n_tok_tiles):
                dump(acc[:, t, :], t * 128, 1024, False)
        checkpoint("p3_%d" % i, _d_p3)

        def _d_p3c():
            for t in range(n_tok_tiles):
                dump(comb[:, t, :], t * 128, 32, False)
        checkpoint("p3c_%d" % i, _d_p3c)

        load_gate(i, 5)
        load_ln(i, 1)
        nsup = 5 if need_ctx else 4
        for ex in range(n_experts):
            g_, j_ = ex // 8, ex % 8
            P.dma("pool", lambda e, g_=g_, j_=j_: e.dma_start(out=gu[:, 0, :, :], in_=I["moe_w_gate"][i, g_, j_].rearrange("(c p) n -> p c n", p=128)), w=["gu0"])
            P.dma("pool", lambda e, g_=g_, j_=j_: e.dma_start(out=gu[:, 1, :, :], in_=I["moe_w_up"][i, g_, j_].rearrange("(c p) n -> p c n", p=128)), w=["gu1"])
            P.dma("pool", lambda e, g_=g_, j_=j_: e.dma_start(out=dr[:], in_=I["moe_w_down"][i, g_, j_].rearrange("(c p) n -> p c n", p=128)), w=["dr"])
            for fc in range(4):
                P.pool(lambda e, fc=fc: e.tensor_tensor(out=dl[:, fc, :], in0=dr[:, fc, :], in1=gb[:, 0, :], op=ALU.mult), r=["dr", "gb"], w=["dl"])
            k = 0
            for fc in range(4):
                for (t0, n) in ST[:nsup]:
                    a = k % 2
                    k += 1
                    pG, pGn, pU, pUn = ps[a], PS[a], ps[2 + a], PS[2 + a]
                    for kc in range(8):
                        P.pe(lambda e, kc=kc, fc=fc, pG=pG, t0=t0, n=n: e.matmul(pG[:, 0:n], lhsT=gu[:, 0, kc, fc * 128:(fc + 1) * 128],
                                                                                  rhs=hT[:, kc, t0:t0 + n], start=(kc == 0), stop=(kc == 7)),
                             r=["gu0"] + hT_names(t0, n), w=[pGn])
                    for kc in range(8):
                        P.pe(lambda e, kc=kc, fc=fc, pU=pU, t0=t0, n=n: e.matmul(pU[:, 0:n], lhsT=gu[:, 1, kc, fc * 128:(fc + 1) * 128],
                                                                                  rhs=hT[:, kc, t0:t0 + n], start=(kc == 0), stop=(kc == 7)),
                             r=["gu1"] + hT_names(t0, n), w=[pUn])
                    hn = "hid_%d" % t0
                    P.act(lambda e, fc=fc, pG=pG, t0=t0, n=n: e.activation(out=hid[:, fc, t0:t0 + n], in_=pG[:, 0:n], func=AF.Silu), r=[pGn], w=[hn])
                    P.dve(lambda e, fc=fc, pU=pU, t0=t0, n=n: e.tensor_tensor(out=hid[:, fc, t0:t0 + n], in0=hid[:, fc, t0:t0 + n], in1=pU[:, 0:n], op=ALU.mult),
                          r=[pUn, hn], w=[hn])
            for t in range(n_tok_tiles):
                lat = t < 16
                wd = dl if lat else dr
                wdn = "dl" if lat else "dr"
                for half in range(2):
                    a = 4 + 2 * (t % 2) + half
                    py, pyn = ps[a], PS[a]
                    for fc in range(4):
                        P.pe(lambda e, fc=fc, py=py, t=t, half=half, wd=wd: e.matmul(py[:], lhsT=hid[:, fc, t * 128:(t + 1) * 128],
                                                                                      rhs=wd[:, fc, half * 512:(half + 1) * 512],
                                                                                      start=(fc == 0), stop=(fc == 3)),
                             r=[wdn, "hid_%d" % ((t // 4) * 512)], w=[pyn])
                    cw = comb[:, t, ex:ex + 1]
                    dst = acc[:, t, half * 512:(half + 1) * 512]
                    if lat:
                        P.dve(lambda e, py=py, cw=cw, dst=dst: e.scalar_tensor_tensor(out=dst, in0=py[:], scalar=cw, in1=dst, op0=ALU.mult, op1=ALU.add),
                              r=[pyn, "comb_%d" % t, "acc_%d" % t], w=["acc_%d" % t])
                    else:
                        tb, tbn = S[half], "S%d" % half
                        P.dve(lambda e, tb=tb, py=py, half=half: e.tensor_tensor(out=tb[:], in0=py[:], in1=gb[:, 1, half * 512:(half + 1) * 512], op=ALU.mult),
                              r=[pyn, "gb"], w=[tbn])
                        P.dve(lambda e, tb=tb, cw=cw, dst=dst: e.scalar_tensor_tensor(out=dst, in0=tb[:], scalar=cw, in1=dst, op0=ALU.mult, op1=ALU.add),
                              r=[tbn, "comb_%d" % t, "acc_%d" % t], w=["acc_%d" % t])
        P.barrier()

    try:
        for i_ in range(n_layers):
            layer(i_)
    except _Stop:
        with nc.allow_non_contiguous_dma(reason="dbg"):
            P.emit()
        return nc

    nfin = 16 if n_layers == DEPTH else NTILE
    for t in range(nfin):
        s = t % 2
        layer_norm_tile(acc[:, t, :], xt[s][:], "acc_%d" % t, "xt%d" % s)
        if n_layers == DEPTH or t < 16:
            P.dma("sp", lambda e, s=s, t=t: e.dma_start(out=out[t * 128:(t + 1) * 128, :], in_=xt[s][:]), r=["xt%d" % s], w=["out_%d" % t])
        if dbg:
            P.dma("sp", lambda e, s=s, t=t: e.dma_start(out=dbg_x[t * 128:(t + 1) * 128, :], in_=xt[s][:]), r=["xt%d" % s], w=["dbg_%d" % t])
    with nc.allow_non_contiguous_dma(reason="small per-partition parameter loads"):
        P.emit()
    return nc


_CONSTS = None


def kernel(**inputs):
    global _CONSTS
    if _CONSTS is None:
        _CONSTS = _consts()
    nc = build_nc()
    shared = {}
    for k, shp in INPUT_SHAPES.items():
        if k in ("x", "c", "ctx"):
            continue
        src = _CONSTS[k] if k.startswith("k_") else inputs[k]
        shared[k] = np.ascontiguousarray(np.asarray(src, dtype=np.float32)).reshape(shp)
    in_maps = []
    for b in range(8):
        m = dict(shared)
        m["x"] = np.ascontiguousarray(np.asarray(inputs["x"][b], dtype=np.float32))
        m["c"] = np.ascontiguousarray(np.asarray(inputs["c"][b], dtype=np.float32))
        m["ctx"] = np.ascontiguousarray(np.asarray(inputs["ctx"][b], dtype=np.float32))
        in_maps.append(m)
    res = run_bass_kernel_spmd(nc, in_maps, core_ids=list(range(8)))
    return np.stack([np.asarray(r["out"], dtype=np.float32) for r in res.results], axis=0)
```

```python
import contextlib
import math
import numpy as np
import concourse.bass as bass
import concourse.mybir as mybir
from concourse.bass_utils import run_bass_kernel_spmd

F32 = mybir.dt.float32
BF16 = mybir.dt.bfloat16
AF = mybir.ActivationFunctionType
ALU = mybir.AluOpType
AX = mybir.AxisListType

D = 1024
SEQ = 2048
NCTX = 256
NT = SEQ + NCTX
NTILE = NT // 128
DEPTH = 4
ALPHA = (2.0 * DEPTH) ** 0.25
LN_EPS = 1e-5
RMS_EPS = 1e-6
THETA = 10000.0
ST = [(0, 512), (512, 512), (1024, 512), (1536, 512), (2048, 256)]

DMA_RING = 8


class _Op:
    __slots__ = ("eng", "fn", "deps", "dma", "sig", "dsem", "dval", "idx")

    def __init__(self, eng, fn, deps, dma, idx):
        self.eng = eng
        self.fn = fn
        self.deps = deps
        self.dma = dma
        self.sig = 0
        self.dsem = None
        self.dval = 0
        self.idx = idx


class Prog:
    ENGS = ("pe", "act", "dve", "pool", "sp")

    def __init__(self, nc):
        self.nc = nc
        self.ops = []
        self.last_w = {}
        self.readers = {}
        self.dma_hist = {e: [] for e in self.ENGS}
        self.last_on = {}
        self.bar = {}

    def add(self, eng, fn, r=(), w=(), dma=False):
        deps = set()
        for b in r:
            lw = self.last_w.get(b)
            if lw is not None:
                deps.add(lw)
        for b in w:
            lw = self.last_w.get(b)
            if lw is not None:
                deps.add(lw)
            for x in self.readers.get(b, ()):
                deps.add(x)
        if eng in self.bar:
            deps |= self.bar.pop(eng)
        idx = len(self.ops)
        op = _Op(eng, fn, deps, dma, idx)
        if dma:
            h = self.dma_hist[eng]
            if len(h) >= DMA_RING:
                deps.add(h[-DMA_RING])
            h.append(idx)
        else:
            self.last_on[eng] = idx
        for b in r:
            self.readers.setdefault(b, []).append(idx)
        for b in w:
            self.last_w[b] = idx
            self.readers[b] = []
        self.ops.append(op)
        return idx

    def barrier(self):
        s = set(self.last_on.values())
        for e in self.ENGS:
            s |= set(self.dma_hist[e][-DMA_RING:])
        for e in self.ENGS:
            self.bar[e] = set(s) | self.bar.get(e, set())

    def pe(self, fn, r=(), w=()):
        return self.add("pe", fn, r, w)

    def act(self, fn, r=(), w=()):
        return self.add("act", fn, r, w)

    def dve(self, fn, r=(), w=()):
        return self.add("dve", fn, r, w)

    def pool(self, fn, r=(), w=()):
        return self.add("pool", fn, r, w)

    def dma(self, eng, fn, r=(), w=()):
        return self.add(eng, fn, r, w, dma=True)

    def emit(self):
        nc = self.nc
        ops = self.ops
        needed = set()
        for op in ops:
            for d in op.deps:
                dop = ops[d]
                if dop.dma:
                    continue
                if dop.eng == "pe" and op.eng == "pe" and not op.dma:
                    continue
                needed.add(d)
        cnt = {e: 0 for e in self.ENGS}
        dcount = {e: 0 for e in self.ENGS}
        last_compute = {}
        for op in ops:
            if op.dma:
                n = dcount[op.eng]
                dcount[op.eng] += 1
                op.dsem = (op.eng, n % DMA_RING)
                op.dval = 16 * (n // DMA_RING + 1)
            else:
                last_compute[op.eng] = op.idx
        for e, i in last_compute.items():
            needed.add(i)
        for op in ops:
            if not op.dma and op.idx in needed:
                cnt[op.eng] += 1
                op.sig = cnt[op.eng]
        with contextlib.ExitStack() as es:
            csem = {e: es.enter_context(nc.semaphore("s_" + e)) for e in self.ENGS}
            dsem = {}
            for e in self.ENGS:
                for k in range(min(DMA_RING, dcount[e])):
                    dsem[(e, k)] = es.enter_context(nc.semaphore("d_%s_%d" % (e, k)))
            block = es.enter_context(nc.Block())
            by_eng = {e: [op for op in ops if op.eng == e] for e in self.ENGS}

            def run(engname, eng):
                waited = {}
                for op in by_eng[engname]:
                    for d in sorted(op.deps):
                        dop = ops[d]
                        if dop.dma:
                            sem, val, key = dsem[dop.dsem], dop.dval, ("d",) + dop.dsem
                        else:
                            if dop.eng == "pe" and engname == "pe" and not op.dma:
                                continue
                            sem, val, key = csem[dop.eng], dop.sig, ("c", dop.eng)
                        if waited.get(key, 0) >= val:
                            continue
                        waited[key] = val
                        eng.wait_ge(sem, val)
                    ins = op.fn(eng)
                    if op.dma:
                        ins.then_inc(dsem[op.dsem], 16)
                    elif op.sig:
                        ins.then_inc(csem[engname], 1)
                if engname == "sp":
                    for e in self.ENGS:
                        if cnt[e] > 0 and e != "sp":
                            eng.wait_ge(csem[e], cnt[e])
                    for e in self.ENGS:
                        n = dcount[e]
                        for k in range(min(DMA_RING, n)):
                            last = ((n - 1 - k) // DMA_RING) * DMA_RING + k
                            eng.wait_ge(dsem[(e, k)], 16 * (last // DMA_RING + 1))

            @block.tensor
            def _(eng):
                run("pe", eng)

            @block.scalar
            def _(eng):
                run("act", eng)

            @block.vector
            def _(eng):
                run("dve", eng)

            @block.gpsimd
            def _(eng):
                run("pool", eng)

            @block.sync
            def _(eng):
                run("sp", eng)


def _consts():
    ident = np.eye(128, dtype=np.float32)

    def rmat(hd):
        nf = hd // 4
        R = np.zeros((128, 128), np.float32)
        for b in range(0, 128, hd):
            for ax in range(2):
                o = b + ax * 2 * nf
                for f in range(nf):
                    R[o + f, o + nf + f] = -1.0
                    R[o + nf + f, o + f] = 1.0
        return np.ascontiguousarray(R.T)

    def tab(hd):
        nf = hd // 4
        t = np.arange(SEQ)
        row = (t // 64).astype(np.float32)
        col = (t % 64).astype(np.float32)
        inv = (np.float32(THETA) ** (-np.arange(nf, dtype=np.float32) / np.float32(nf))).astype(np.float32)
        out = np.zeros((2, 128, SEQ), np.float32)
        for p in range(128):
            d = p % hd
            ax = d // (2 * nf)
            f = d % nf
            pos = row if ax == 0 else col
            ang = (pos * inv[f]).astype(np.float32)
            out[0, p] = np.cos(ang)
            out[1, p] = np.sin(ang)
        return out

    mask = np.zeros((128, 6, 512), np.float32)
    k = np.arange(128)[:, None]
    q = np.arange(512)[None, :]
    for ri in range(6):
        rel = ri - 1
        mask[:, ri, :] = (np.abs(q - rel * 128 - k) <= 128).astype(np.float32)
    return {"k_ident": ident, "k_rt128": rmat(128), "k_rt64": rmat(64), "k_tabA": tab(128), "k_tabB": tab(64),
            "k_mask": mask}


INPUT_SHAPES = {
    "x": [SEQ, D], "c": [D], "ctx": [NCTX, D], "c_ctx": [D],
    "w_mod": [4, D, 6 * D], "b_mod": [4, 6 * D], "ln_gain": [4, 2, D], "ln_bias": [4, 2, D],
    "a_w_qkv": [2, D, 1536], "a_q_gain": [2, 128], "a_k_gain": [2, 128], "a_w_o": [2, D, D],
    "b_w_qkv": [1, D, 1536], "b_sink": [1, 16], "b_w_o": [1, D, D],
    "c_w_qkv": [1, D, 3072], "c_lam_q1": [1, 64], "c_lam_k1": [1, 64], "c_lam_q2": [1, 64], "c_lam_k2": [1, 64],
    "c_subln_gain": [1, 128], "c_w_o": [1, D, D],
    "moe_w_group": [4, D, 4], "moe_b_group": [4, 4], "moe_w_expert": [4, D, 32], "moe_b_expert": [4, 32],
    "moe_w_gate": [4, 4, 8, D, 512], "moe_w_up": [4, 4, 8, D, 512], "moe_w_down": [4, 4, 8, 512, D],
    "k_ident": [128, 128], "k_rt128": [128, 128], "k_rt64": [128, 128], "k_tabA": [2, 128, SEQ],
    "k_tabB": [2, 128, SEQ], "k_mask": [128, 6, 512],
}


class _Stop(Exception):
    pass


def build_nc(n_layers=DEPTH, n_experts=32, dbg=False, stop=None):
    nc = bass.Bass("TRN2", target_bir_lowering=False)
    I = {k: nc.dram_tensor(k, v, F32, kind="ExternalInput").ap() for k, v in INPUT_SHAPES.items()}
    out = nc.dram_tensor("out", [SEQ, D], F32, kind="ExternalOutput").ap()
    dbg_x = nc.dram_tensor("dbg_x", [NT, D], F32, kind="ExternalOutput").ap() if dbg else None
    xs = nc.dram_tensor("xs_scr", [NT, D], F32, kind="Internal").ap()
    md = nc.dram_tensor("md_scr", [4, 2, 6 * D], F32, kind="Internal").ap()

    base = [16512]

    def alloc(name, shape, dt, off=None):
        nbytes = int(np.prod(shape[1:])) * (4 if dt == F32 else 2)
        if off is None:
            off = base[0]
            base[0] += (nbytes + 63) // 64 * 64
        return nc.alloc_sbuf_tensor_at(name, shape, dt, offset=off)

    identF = alloc("identF", [128, 128], F32)
    ones_bf = alloc("ones_bf", [128, 128], BF16)
    rt128 = alloc("rt128", [128, 128], BF16)
    rt64 = alloc("rt64", [128, 128], BF16)
    cT = alloc("cT", [128, 8, 2], F32)
    sT = alloc("sT", [128, 8, 2], F32)
    modT = alloc("modT", [128, 8, 8], F32)
    small = alloc("small", [128, 64], F32)
    lamv = alloc("lamv", [128, 4, 64], F32)
    esb = alloc("esb", [128, 16], F32)
    lnp = alloc("lnp", [128, 2, D], F32)
    gb = alloc("gb", [128, 2, D], F32)
    wr = alloc("wr", [128, 8, 36], F32)
    brb = alloc("brb", [128, 36], F32)
    comb = alloc("comb", [128, NTILE, 32], F32)
    rscr = alloc("rscr", [128, 208], F32)
    onesF = alloc("onesF", [1, 128], F32)
    W0 = base[0]
    KB = 1024
    acc = alloc("acc", [128, NTILE, D], F32, W0)
    kT = alloc("kT", [128, 4, NT], BF16, W0)
    Vb = alloc("Vb", [128, NTILE, 512], BF16, W0 + 18 * KB)
    maskb = alloc("maskb", [128, 6, 512], BF16, W0 + 36 * KB)
    cosT = alloc("cosT", [128, SEQ], F32, W0 + 42 * KB)
    sinT = alloc("sinT", [128, SEQ], F32, W0 + 50 * KB)
    wv = alloc("wv", [128, 8, 512], BF16, W0 + 58 * KB)
    hT = alloc("hT", [128, 8, NT], BF16, W0 + 72 * KB)
    qT = alloc("qT", [128, 8, NT], BF16, W0 + 108 * KB)
    gu = alloc("gu", [128, 2, 8, 512], BF16, W0 + 108 * KB)
    dr = alloc("dr", [128, 4, D], BF16, W0 + 124 * KB)
    dl = alloc("dl", [128, 4, D], BF16, W0 + 132 * KB)
    M0 = W0 + 144 * KB
    wring = [alloc("wring%d" % i, [128, 8, 128], BF16, M0 + 2 * KB * i) for i in range(4)]
    w_o = alloc("w_o", [128, 8, D], BF16, M0)
    hid = alloc("hid", [128, 4, NT], BF16, M0)
    S = [alloc("S%d" % i, [128, 512], F32, M0 + 18 * KB + 2 * KB * i) for i in range(5)]
    hTf = alloc("hTf", [128, 8, 128], F32, M0 + 18 * KB + 2 * KB * 3)
    qz = alloc("qz", [128, 2, 512], BF16, M0 + 18 * KB + 2 * KB * 4)
    Bs = [alloc("B%d" % i, [128, 512], BF16, M0 + 28 * KB + KB * i) for i in range(4)]
    xt = [alloc("xt%d" % i, [128, D], F32, M0 + 32 * KB + 4 * KB * i) for i in range(2)]
    end = M0 + 40 * KB
    assert end <= 229344, end
    wm = [alloc("wm%d" % i, [128, 8, 512], F32, W0 + 16 * KB * i) for i in range(2)]
    m_sb = alloc("m_sb", [2, 6 * D], F32, W0 + 32 * KB)
    bm = alloc("bm", [2, 6 * D], F32, W0 + 56 * KB)

    ps = [nc.alloc_psum_tensor("ps%d" % i, [128, 512], F32) for i in range(8)]
    PS = ["ps%d" % i for i in range(8)]

    P = Prog(nc)
    dcnt = [0]

    def dump(ap, row0, n, is_bf=True):
        k = dcnt[0] % 2
        dcnt[0] += 1
        P.barrier()
        P.act(lambda e: e.activation(out=xt[k][:, 0:n], in_=ap, func=AF.Identity), w=["xt%d" % k])
        P.dma("sp", lambda e: e.dma_start(out=dbg_x[row0:row0 + 128, 0:n], in_=xt[k][:, 0:n]), r=["xt%d" % k], w=["dbgd"])
        P.barrier()

    def checkpoint(name, fn):
        if stop == name:
            P.barrier()
            fn()
            raise _Stop()

    P.dma("sp", lambda e: e.dma_start(out=identF[:], in_=I["k_ident"]), w=["identF"])
    P.dma("pool", lambda e: e.dma_start(out=rt128[:], in_=I["k_rt128"]), w=["rt128"])
    P.dma("pool", lambda e: e.dma_start(out=rt64[:], in_=I["k_rt64"]), w=["rt64"])
    P.dve(lambda e: e.memset(ones_bf[:], 1.0), w=["ones_bf"])
    P.dve(lambda e: e.memset(onesF[:], 1.0), w=["onesF"])
    P.dma("sp", lambda e: e.dma_start(out=cT[:, :, 0], in_=I["c"].rearrange("(c p) -> p c", p=128)), w=["cT"])
    P.dma("sp", lambda e: e.dma_start(out=cT[:, :, 1], in_=I["c_ctx"].rearrange("(c p) -> p c", p=128)), w=["cT"])
    P.act(lambda e: e.activation(out=sT[:], in_=cT[:], func=AF.Silu), r=["cT"], w=["sT"])

    cnt = 0
    for i in range(n_layers):
        for r_ in range(2):
            P.dma("sp", lambda e, i=i, r_=r_: e.dma_start(out=bm[r_:r_ + 1, :], in_=I["b_mod"][i:i + 1, :]), w=["bm"])
        for j in range(12):
            s = cnt % 2
            cnt += 1
            P.dma("sp", lambda e, i=i, j=j, s=s: e.dma_start(
                out=wm[s][:], in_=I["w_mod"][i][:, j * 512:(j + 1) * 512].rearrange("(c p) n -> p c n", p=128)),
                w=["wm%d" % s])
            pb = ps[j % 2]
            for kc in range(8):
                P.pe(lambda e, s=s, kc=kc, pb=pb: e.matmul(pb[0:2, :], lhsT=sT[:, kc, :], rhs=wm[s][:, kc, :],
                                                           start=(kc == 0), stop=(kc == 7)),
                     r=["sT", "wm%d" % s], w=[PS[j % 2]])
            P.dve(lambda e, j=j, pb=pb: e.tensor_tensor(out=m_sb[0:2, j * 512:(j + 1) * 512], in0=pb[0:2, :],
                                                        in1=bm[0:2, j * 512:(j + 1) * 512], op=ALU.add),
                  r=[PS[j % 2], "bm"], w=["m_sb"])
        P.dma("sp", lambda e, i=i: e.dma_start(out=md[i], in_=m_sb[0:2, :]), r=["m_sb"], w=["md%d" % i])
    P.barrier()

    def _d_pro():
        P.dma("sp", lambda e: e.dma_start(out=dbg_x[0:12, :], in_=md[0].rearrange("r (a d) -> (r a) d", d=D)), r=["md0"], w=["dbgd"])
    try:
        checkpoint("pro", _d_pro)
    except _Stop:
        with nc.allow_non_contiguous_dma(reason="dbg"):
            P.emit()
        return nc

    def bcast_row(dst_fn, n, src_row, dst_names):
        P.dma("sp", lambda e: e.dma_start(out=xt[1][0:1, 0:n], in_=src_row.rearrange("(o n) -> o n", o=1)), w=["xt1"])
        for c0 in range(0, n, 512):
            w_ = min(512, n - c0)
            P.pe(lambda e, c0=c0, w_=w_: e.matmul(ps[7][:, 0:w_], lhsT=onesF[0:1, :], rhs=xt[1][0:1, c0:c0 + w_], start=True, stop=True),
                 r=["xt1", "onesF"], w=[PS[7]])
            P.act(lambda e, c0=c0, w_=w_: e.activation(out=dst_fn(c0, w_), in_=ps[7][:, 0:w_], func=AF.Identity), r=[PS[7]], w=dst_names)

    def load_mod(i):
        for slot, (v, row) in enumerate([(0, 0), (1, 0), (0, 1), (1, 1), (3, 0), (4, 0), (3, 1), (4, 1)]):
            P.dma("sp", lambda e, slot=slot, v=v, row=row: e.dma_start(
                out=modT[:, slot, :], in_=md[i][row, v * D:(v + 1) * D].rearrange("(c p) -> p c", p=128)),
                r=["md%d" % i], w=["modT"])
        for slot in (1, 3, 5, 7):
            P.dve(lambda e, slot=slot: e.tensor_scalar(out=modT[:, slot, :], in0=modT[:, slot, :], scalar1=1.0,
                                                       scalar2=None, op0=ALU.add), r=["modT"], w=["modT"])

    def load_gate(i, v):
        for row in range(2):
            bcast_row(lambda c0, w_, row=row: gb[:, row, c0:c0 + w_], D, md[i][row, v * D:(v + 1) * D], ["gb"])

    def load_ln(i, j):
        bcast_row(lambda c0, w_: lnp[:, 0, c0:c0 + w_], D, I["ln_gain"][i, j, :], ["lnp"])
        bcast_row(lambda c0, w_: lnp[:, 1, c0:c0 + w_], D, I["ln_bias"][i, j, :], ["lnp"])

    def layer_norm_tile(src, dst, nm_src, nm_dst):
        st6 = rscr[:, 0:12].rearrange("p (a b) -> p a b", a=2)
        mv = rscr[:, 12:14]
        for h in range(2):
            P.dve(lambda e, h=h: e.bn_stats(out=st6[:, h, :], in_=src[:, h * 512:(h + 1) * 512]), r=[nm_src], w=["st6"])
        P.dve(lambda e: e.bn_aggr(out=mv, in_=rscr[:, 0:12]), r=["st6"], w=["mv"])
        P.act(lambda e: e.activation(out=rscr[:, 14:15], in_=rscr[:, 13:14], func=AF.Ln, bias=small[:, 0:1], scale=1.0),
              r=["mv", "small"], w=["sd"])
        P.act(lambda e: e.activation(out=rscr[:, 15:16], in_=rscr[:, 14:15], func=AF.Exp, scale=-0.5), r=["sd"], w=["rstd"])
        P.dve(lambda e: e.tensor_scalar(out=rscr[:, 16:17], in0=rscr[:, 12:13], scalar1=rscr[:, 15:16], scalar2=-1.0,
                                        op0=ALU.mult, op1=ALU.mult), r=["mv", "rstd"], w=["nb"])
        P.act(lambda e: e.activation(out=dst, in_=src, func=AF.Identity, scale=rscr[:, 15:16], bias=rscr[:, 16:17]),
              r=[nm_src, "rstd", "nb"], w=[nm_dst])
        P.pool(lambda e: e.tensor_tensor(out=dst, in0=dst, in1=lnp[:, 0, :], op=ALU.mult), r=[nm_dst, "lnp"], w=[nm_dst])
        P.pool(lambda e: e.tensor_tensor(out=dst, in0=dst, in1=lnp[:, 1, :], op=ALU.add), r=[nm_dst, "lnp"], w=[nm_dst])

    def build_hT_tile(src, nm_src, t, sc_slot, sh_slot, with_f32, pb2):
        for c in range(8):
            pb = ps[pb2 + c // 4]
            P.pe(lambda e, c=c, pb=pb: e.transpose(out=pb[:, (c % 4) * 128:(c % 4 + 1) * 128],
                                                   in_=src[:, c * 128:(c + 1) * 128], identity=identF[:]),
                 r=[nm_src, "identF"], w=[PS[pb2 + c // 4]])
        for c in range(8):
            pb = ps[pb2 + c // 4]
            pin = pb[:, (c % 4) * 128:(c % 4 + 1) * 128]
            dst = hT[:, c, t * 128:(t + 1) * 128]
            if with_f32:
                P.dve(lambda e, pin=pin, c=c: e.tensor_scalar(out=hTf[:, c, :], in0=pin, scalar1=modT[:, sc_slot, c:c + 1],
                                                              scalar2=modT[:, sh_slot, c:c + 1], op0=ALU.mult, op1=ALU.add),
                      r=[PS[pb2 + c // 4], "modT"], w=["hTf_%d" % c])
                P.act(lambda e, dst=dst, c=c: e.activation(out=dst, in_=hTf[:, c, :], func=AF.Identity),
                      r=["hTf_%d" % c], w=["hT_%d" % t])
            elif c % 2 == 0:
                P.act(lambda e, pin=pin, dst=dst, c=c: e.activation(out=dst, in_=pin, func=AF.Identity,
                                                                    scale=modT[:, sc_slot, c:c + 1],
                                                                    bias=modT[:, sh_slot, c:c + 1]),
                      r=[PS[pb2 + c // 4], "modT"], w=["hT_%d" % t])
            else:
                P.dve(lambda e, pin=pin, dst=dst, c=c: e.tensor_scalar(out=dst, in0=pin, scalar1=modT[:, sc_slot, c:c + 1],
                                                                       scalar2=modT[:, sh_slot, c:c + 1], op0=ALU.mult,
                                                                       op1=ALU.add),
                      r=[PS[pb2 + c // 4], "modT"], w=["hT_%d" % t])

    def hT_names(t0, n):
        return ["hT_%d" % t for t in range(t0 // 128, (t0 + n) // 128)]

    P.dve(lambda e: e.memset(small[:, 0:1], LN_EPS), w=["small"])
    P.dve(lambda e: e.memset(small[:, 1:2], RMS_EPS), w=["small"])

    def layer(i):
        kind, jj = i % 3, i // 3
        last = (i == DEPTH - 1)
        need_ctx = not last
        n_tok_tiles = NTILE if need_ctx else 16
        wqkv = [I["a_w_qkv"], I["b_w_qkv"], I["c_w_qkv"]][kind][jj]
        wo_d = [I["a_w_o"], I["b_w_o"], I["c_w_o"]][kind][jj]
        hd = 128 if kind == 0 else 64
        scale = hd ** -0.5
        rt = rt128 if kind == 0 else rt64
        rtn = "rt128" if kind == 0 else "rt64"
        tabd = I["k_tabA"] if kind == 0 else I["k_tabB"]

        load_mod(i)
        if i > 0:
            P.barrier()
        def p1a_a(t):
            s = t % 2
            if i == 0:
                src_d = I["x"][t * 128:(t + 1) * 128, :] if t < 16 else I["ctx"][(t - 16) * 128:(t - 15) * 128, :]
                P.dma("sp", lambda e, s=s, src_d=src_d: e.dma_start(out=xt[s][:], in_=src_d), w=["xt%d" % s])
            else:
                layer_norm_tile(acc[:, t, :], xt[s][:], "acc_%d" % t, "xt%d" % s)
                P.dma("sp", lambda e, s=s, t=t: e.dma_start(out=xs[t * 128:(t + 1) * 128, :], in_=xt[s][:]),
                      r=["xt%d" % s], w=["xs_%d" % t])

        def p1a_b(t):
            s = t % 2
            lat = t < 16
            build_hT_tile(xt[s][:], "xt%d" % s, t, 1 if lat else 3, 0 if lat else 2, False, 2 * (t % 2))

        for t in range(NTILE + 1):
            if t < NTILE:
                p1a_a(t)
            if t >= 1:
                p1a_b(t - 1)
        P.barrier()

        def _d_p1a():
            for c in range(8):
                dump(hT[:, c, 0:1024], c * 128, 1024)
            for c in range(8):
                dump(hT[:, c, 2048:2304], 1024 + c * 128, 256)
        checkpoint("p1a_%d" % i, _d_p1a)

        P.dma("sp", lambda e: e.dma_start(out=cosT[:], in_=tabd[0]), w=["cosT"])
        P.dma("sp", lambda e: e.dma_start(out=sinT[:], in_=tabd[1]), w=["sinT"])
        if kind == 0:
            P.dma("sp", lambda e: e.dma_start(out=small[:, 2:3], in_=I["a_q_gain"][jj].rearrange("(p o) -> p o", o=1)), w=["small"])
            P.dma("sp", lambda e: e.dma_start(out=small[:, 3:4], in_=I["a_k_gain"][jj].rearrange("(p o) -> p o", o=1)), w=["small"])
        elif kind == 1:
            bcast_row(lambda c0, w_: esb[:, c0:c0 + w_], 16, I["b_sink"][0, :], ["esb"])
            P.act(lambda e: e.activation(out=esb[:], in_=esb[:], func=AF.Exp), r=["esb"], w=["esb"])
            P.dma("pool", lambda e: e.dma_start(out=maskb[:], in_=I["k_mask"]), w=["maskb"])
        else:
            lam_init = 0.8 - 0.6 * math.exp(-0.3 * i)
            for q_, nm in enumerate(["c_lam_q1", "c_lam_k1", "c_lam_q2", "c_lam_k2"]):
                bcast_row(lambda c0, w_, q_=q_: lamv[:, q_, c0:c0 + w_], 64, I[nm][0, :], ["lamv"])
            P.dve(lambda e: e.tensor_tensor(out=lamv[:, 0, :], in0=lamv[:, 0, :], in1=lamv[:, 1, :], op=ALU.mult), r=["lamv"], w=["lamv"])
            P.dve(lambda e: e.tensor_tensor(out=lamv[:, 2, :], in0=lamv[:, 2, :], in1=lamv[:, 3, :], op=ALU.mult), r=["lamv"], w=["lamv"])
            P.dve(lambda e: e.tensor_reduce(out=small[:, 4:5], in_=lamv[:, 0, :], axis=AX.X, op=ALU.add), r=["lamv"], w=["small"])
            P.dve(lambda e: e.tensor_reduce(out=small[:, 5:6], in_=lamv[:, 2, :], axis=AX.X, op=ALU.add), r=["lamv"], w=["small"])
            P.act(lambda e: e.activation(out=small[:, 4:6], in_=small[:, 4:6], func=AF.Exp), r=["small"], w=["small"])
            P.dve(lambda e: e.tensor_tensor(out=small[:, 6:7], in0=small[:, 5:6], in1=small[:, 4:5], op=ALU.subtract), r=["small"], w=["small"])
            P.dve(lambda e: e.tensor_scalar(out=small[:, 6:7], in0=small[:, 6:7], scalar1=-lam_init, scalar2=None, op0=ALU.add),
                  r=["small"], w=["small"])
            P.dma("sp", lambda e: e.dma_start(out=small[:, 7:8], in_=I["c_subln_gain"][0].rearrange("(p o) -> p o", o=1)), w=["small"])
            P.dve(lambda e: e.tensor_scalar(out=small[:, 7:8], in0=small[:, 7:8], scalar1=1.0 - lam_init, scalar2=None, op0=ALU.mult),
                  r=["small"], w=["small"])

        sidx = [0]
        bidx = [0]
        if kind != 0:
            P.pool(lambda e: e.memset(qz[64:128, 0, :], 0.0), w=["S4"])
            P.pool(lambda e: e.memset(qz[0:64, 1, :], 0.0), w=["S4"])

        def fill_qz(src_lo, src_hi, n, rnames):
            if src_lo is not None:
                P.dve(lambda e: e.tensor_copy(out=qz[0:64, 0, 0:n], in_=src_lo), r=rnames, w=["S4"])
            if src_hi is not None:
                P.dve(lambda e: e.tensor_copy(out=qz[64:128, 1, 0:n], in_=src_hi), r=rnames, w=["S4"])

        def nS():
            sidx[0] = (sidx[0] + 1) % (5 if kind == 0 else 4)
            return S[sidx[0]], "S%d" % sidx[0]

        def nB():
            bidx[0] = (bidx[0] + 1) % 4
            return Bs[bidx[0]], "B%d" % bidx[0]

        wcnt = [0]

        def load_wchunk(cols_list):
            s = wcnt[0] % 4
            wcnt[0] += 1
            for (d0, s0, n) in cols_list:
                P.dma("pool", lambda e, s=s, d0=d0, s0=s0, n=n: e.dma_start(
                    out=wring[s][:, :, d0:d0 + n], in_=wqkv[:, s0:s0 + n].rearrange("(c p) n -> p c n", p=128)),
                    w=["wring%d" % s])
            return wring[s], "wring%d" % s

        pcnt = [0]

        def proj_fm(wt, wnm, dstT, dst_c, dst_nm, mode):
            for (t0, n) in ST:
                if t0 >= SEQ and False:
                    continue
                is_ctx = t0 >= SEQ
                a = pcnt[0] % 2
                pcnt[0] += 1
                pA, nA = ps[a], PS[a]
                pB, nB_ = ps[2 + a], PS[2 + a]
                pC, nC = ps[4 + a], PS[4 + a]
                for kc in range(8):
                    P.pe(lambda e, kc=kc, pA=pA, t0=t0, n=n: e.matmul(pA[:, 0:n], lhsT=wt[:, kc, :], rhs=hT[:, kc, t0:t0 + n],
                                                                     start=(kc == 0), stop=(kc == 7)),
                         r=[wnm] + hT_names(t0, n), w=[nA])
                dst = dstT[:, dst_c, t0:t0 + n]
                dnm = "%s_%d_%d" % (dst_nm, dst_c, t0)
                if mode in ("A_q", "A_k"):
                    gcol = small[:, 2:3] if mode == "A_q" else small[:, 3:4]
                    sq, sqn = nB()
                    P.act(lambda e, sq=sq, pA=pA, n=n: e.activation(out=sq[:, 0:n], in_=pA[:, 0:n], func=AF.Square), r=[nA], w=[sqn])
                    P.pe(lambda e, sq=sq, pB=pB, n=n: e.matmul(pB[:, 0:n], lhsT=ones_bf[:], rhs=sq[:, 0:n], start=True, stop=True),
                         r=[sqn, "ones_bf"], w=[nB_])
                    qg, qgn = nB()
                    P.act(lambda e, qg=qg, pA=pA, n=n, gcol=gcol: e.activation(out=qg[:, 0:n], in_=pA[:, 0:n], func=AF.Identity, scale=gcol),
                          r=[nA, "small"], w=[qgn])
                    rs, rsn = nS()
                    P.act(lambda e, rs=rs, pB=pB, n=n: e.activation(out=rs[:, 0:n], in_=pB[:, 0:n], func=AF.Ln, scale=1.0 / 128,
                                                                   bias=small[:, 1:2]), r=[nB_, "small"], w=[rsn])
                    P.act(lambda e, rs=rs, n=n: e.activation(out=rs[:, 0:n], in_=rs[:, 0:n], func=AF.Exp, scale=-0.5), r=[rsn], w=[rsn])
                    if not is_ctx:
                        P.pe(lambda e, qg=qg, pC=pC, n=n: e.matmul(pC[:, 0:n], lhsT=rt[:], rhs=qg[:, 0:n], start=True, stop=True),
                             r=[qgn, rtn], w=[nC])
                        t1, t1n = nS()
                        P.pool(lambda e, t1=t1, qg=qg, n=n, t0=t0: e.tensor_tensor(out=t1[:, 0:n], in0=qg[:, 0:n], in1=cosT[:, t0:t0 + n], op=ALU.mult),
                               r=[qgn, "cosT"], w=[t1n])
                        t2, t2n = nS()
                        P.dve(lambda e, t2=t2, pC=pC, n=n, t0=t0: e.tensor_tensor(out=t2[:, 0:n], in0=pC[:, 0:n], in1=sinT[:, t0:t0 + n], op=ALU.mult),
                              r=[nC, "sinT"], w=[t2n])
                        P.dve(lambda e, t1=t1, t2=t2, n=n: e.tensor_tensor(out=t1[:, 0:n], in0=t1[:, 0:n], in1=t2[:, 0:n], op=ALU.add),
                              r=[t1n, t2n], w=[t1n])
                        P.dve(lambda e, t1=t1, rs=rs, n=n, dst=dst: e.tensor_tensor(out=dst, in0=t1[:, 0:n], in1=rs[:, 0:n], op=ALU.mult),
                              r=[t1n, rsn], w=[dnm])
                    else:
                        P.dve(lambda e, qg=qg, rs=rs, n=n, dst=dst: e.tensor_tensor(out=dst, in0=qg[:, 0:n], in1=rs[:, 0:n], op=ALU.mult),
                              r=[qgn, rsn], w=[dnm])
                else:
                    if not is_ctx:
                        qb, qbn = nB()
                        P.act(lambda e, qb=qb, pA=pA, n=n: e.activation(out=qb[:, 0:n], in_=pA[:, 0:n], func=AF.Identity), r=[nA], w=[qbn])
                        P.pe(lambda e, qb=qb, pC=pC, n=n: e.matmul(pC[:, 0:n], lhsT=rt[:], rhs=qb[:, 0:n], start=True, stop=True),
                             r=[qbn, rtn], w=[nC])
                        t1, t1n = nS()
                        P.dve(lambda e, t1=t1, pA=pA, n=n, t0=t0: e.tensor_tensor(out=t1[:, 0:n], in0=pA[:, 0:n], in1=cosT[:, t0:t0 + n], op=ALU.mult),
                              r=[nA, "cosT", qbn], w=[t1n])
                        t2, t2n = nS()
                        P.dve(lambda e, t2=t2, pC=pC, n=n, t0=t0: e.tensor_tensor(out=t2[:, 0:n], in0=pC[:, 0:n], in1=sinT[:, t0:t0 + n], op=ALU.mult),
                              r=[nC, "sinT"], w=[t2n])
                        P.pool(lambda e, t1=t1, t2=t2, n=n, dst=dst: e.tensor_tensor(out=dst, in0=t1[:, 0:n], in1=t2[:, 0:n], op=ALU.add),
                               r=[t1n, t2n], w=[dnm])
                    else:
                        P.act(lambda e, pA=pA, n=n, dst=dst: e.activation(out=dst, in_=pA[:, 0:n], func=AF.Identity), r=[nA], w=[dnm])

        def names_fm(nm, c, t0, n):
            res = []
            for (a, b) in ST:
                if a < t0 + n and t0 < a + b:
                    res.append("%s_%d_%d" % (nm, c, a))
            return res

        for g in range(2):
            if kind == 0:
                for j in range(4):
                    wt, wnm = load_wchunk([(0, (4 * g + j) * 128, 128)])
                    proj_fm(wt, wnm, qT, 4 * g + j, "qT", "A_q")
                wt, wnm = load_wchunk([(0, 1024 + g * 128, 128)])
                proj_fm(wt, wnm, kT, 0, "kT", "A_k")
                vcols, vsrc = 128, 1280 + g * 128
            elif kind == 1:
                for j in range(4):
                    wt, wnm = load_wchunk([(0, (4 * g + j) * 128, 128)])
                    proj_fm(wt, wnm, qT, 4 * g + j, "qT", "rope")
                for j in range(2):
                    c0 = 1024 + (2 * g + j) * 64
                    wt, wnm = load_wchunk([(0, c0, 64), (64, c0, 64)])
                    proj_fm(wt, wnm, kT, j, "kT", "rope")
                vcols, vsrc = 128, 1280 + (2 * g) * 64
            else:
                for j in range(4):
                    wt, wnm = load_wchunk([(0, (4 * g + j) * 128, 128)])
                    proj_fm(wt, wnm, qT, 4 * g + j, "qT", "rope")
                for j in range(4):
                    wt, wnm = load_wchunk([(0, 1024 + (4 * g + j) * 128, 128)])
                    proj_fm(wt, wnm, kT, j, "kT", "rope")
                vcols, vsrc = 512, 2048 + 4 * g * 128
            P.dma("pool", lambda e, vcols=vcols, vsrc=vsrc: e.dma_start(
                out=wv[:, :, 0:vcols], in_=wqkv[:, vsrc:vsrc + vcols].rearrange("(c p) n -> p c n", p=128)), w=["wv"])
            if kind == 1:
                P.pool(lambda e: e.memset(Vb[:], 1.0), w=["Vb_%d" % t for t in range(NTILE)])
            for t in range(NTILE):
                pv, pvn = ps[6 + t % 2], PS[6 + t % 2]
                for kc in range(8):
                    P.pe(lambda e, kc=kc, pv=pv, t=t, vcols=vcols: e.matmul(pv[:, 0:vcols], lhsT=hT[:, kc, t * 128:(t + 1) * 128],
                                                                             rhs=wv[:, kc, 0:vcols], start=(kc == 0), stop=(kc == 7)),
                         r=["wv", "hT_%d" % t], w=[pvn])
                if kind == 1:
                    P.act(lambda e, pv=pv, t=t: e.activation(out=Vb[:, t, 0:256].rearrange("p (h d) -> p h d", h=2)[:, :, 0:64],
                                                             in_=pv[:, 0:128].rearrange("p (h d) -> p h d", h=2), func=AF.Identity),
                          r=[pvn], w=["Vb_%d" % t])
                else:
                    P.act(lambda e, pv=pv, t=t, vcols=vcols: e.activation(out=Vb[:, t, 0:vcols], in_=pv[:, 0:vcols], func=AF.Identity),
                          r=[pvn], w=["Vb_%d" % t])

            def _d_p1b():
                for j in range(4):
                    dump(qT[:, 4 * g + j, 0:1024], j * 128, 1024)
                for j in range(4):
                    dump(kT[:, j, 0:1024], 512 + j * 128, 1024)
                for j in range(4):
                    dump(qT[:, 4 * g + j, 2048:2304], 1024 + j * 128, 256)
                for j in range(4):
                    dump(kT[:, j, 2048:2304], 1536 + j * 128, 256)
                dump(Vb[:, 0, :], 2048, 512)
                dump(Vb[:, 17, :], 2176, 512)
            checkpoint("p1b_%d_%d" % (i, g), _d_p1b)

            if g == 1:
                P.dma("pool", lambda e: e.dma_start(out=w_o[:], in_=wo_d.rearrange("(c p) n -> p c n", p=128)),
                      w=["w_o", "wring0", "wring1", "wring2", "wring3"])

            acnt = [0]

            def attend(q_aps, k_fn, v_fn, qnames, t0, n, kcs, out_fn, sink_h=None, kmask=None, krange=None):
                nm = len(q_aps)
                a = acnt[0] % 2
                acnt[0] += 1
                if nm == 1:
                    pS = [[ps[0], ps[1], ps[2], ps[3]]]
                    nSn = [[PS[0], PS[1], PS[2], PS[3]]]
                    pO, nO = [ps[4 + a]], [PS[4 + a]]
                    pZ, nZ = [ps[6 + a]], [PS[6 + a]]
                else:
                    pS = [[ps[0], ps[1]], [ps[2], ps[3]]]
                    nSn = [[PS[0], PS[1]], [PS[2], PS[3]]]
                    pO, nO = [ps[4], ps[5]], [PS[4], PS[5]]
                    pZ, nZ = [ps[6], ps[7]], [PS[6], PS[7]]
                steps = [(ki, kc, m) for ki, kc in enumerate(kcs) for m in range(nm)]
                pts = {}

                def qk(ki, kc, m):
                    c0, c1 = krange(kc) if krange is not None else (0, n)
                    pSb = pS[m][ki % len(pS[m])]
                    pSn = nSn[m][ki % len(pS[m])]
                    kap, knames = k_fn(m, kc)
                    P.pe(lambda e, pSb=pSb, kap=kap, m=m: e.matmul(pSb[:, c0:c1], lhsT=kap, rhs=q_aps[m][:, c0:c1], start=True, stop=True),
                         r=knames + qnames, w=[pSn])
                    pt, ptn = nB()
                    P.act(lambda e, pt=pt, pSb=pSb: e.activation(out=pt[:, c0:c1], in_=pSb[:, c0:c1], func=AF.Exp, scale=scale),
                          r=[pSn], w=[ptn])
                    if kmask is not None and kmask(kc) is not None:
                        ri = kmask(kc)
                        P.dve(lambda e, pt=pt, ri=ri: e.tensor_tensor(out=pt[:, c0:c1], in0=pt[:, c0:c1], in1=maskb[:, ri, c0:c1], op=ALU.mult),
                              r=[ptn, "maskb"], w=[ptn])
                    pts[(ki, m)] = (pt, ptn, c0, c1)

                def pv(ki, kc, m):
                    pt, ptn, c0, c1 = pts.pop((ki, m))
                    vap, vnames = v_fn(kc)
                    P.pe(lambda e, pt=pt, vap=vap, m=m, ki=ki: e.matmul(pO[m][:, c0:c1], lhsT=vap, rhs=pt[:, c0:c1],
                                                                      start=(ki == 0), stop=(ki == len(kcs) - 1)),
                         r=[ptn] + vnames, w=[nO[m]])
                    if sink_h is None:
                        P.pe(lambda e, pt=pt, m=m, ki=ki: e.matmul(pZ[m][:, c0:c1], lhsT=ones_bf[:], rhs=pt[:, c0:c1],
                                                                  start=(ki == 0), stop=(ki == len(kcs) - 1)),
                             r=[ptn, "ones_bf"], w=[nZ[m]])

                LA = 3
                for s_ in range(len(steps) + LA):
                    if s_ < len(steps):
                        qk(*steps[s_])
                    if s_ >= LA:
                        pv(*steps[s_ - LA])
                out_fn(pO, pZ, nO, nZ)

            lat_kcs = list(range(NTILE))
            ctx_kcs = [16, 17]
            if kind == 0:
                for j in range(4):
                    h = 4 * g + j
                    for (t0, n) in ST:
                        if t0 >= SEQ and not need_ctx:
                            continue
                        kcs = lat_kcs if t0 < SEQ else ctx_kcs

                        def out_fn(pO, pZ, nO, nZ, h=h, t0=t0, n=n):
                            rz, rzn = nS()
                            P.act(lambda e: e.activation(out=rz[:, 0:n], in_=pZ[0][:, 0:n], func=AF.Ln), r=[nZ[0]], w=[rzn])
                            P.act(lambda e: e.activation(out=rz[:, 0:n], in_=rz[:, 0:n], func=AF.Exp, scale=-1.0), r=[rzn], w=[rzn])
                            P.dve(lambda e: e.tensor_tensor(out=qT[:, h, t0:t0 + n], in0=pO[0][:, 0:n], in1=rz[:, 0:n], op=ALU.mult),
                                  r=[nO[0], rzn], w=["qT_%d_%d" % (h, t0)])

                        attend([qT[:, h, t0:t0 + n]],
                               lambda m, kc: (kT[:, 0, kc * 128:(kc + 1) * 128], names_fm("kT", 0, kc * 128, 128)),
                               lambda kc: (Vb[:, kc, 0:128], ["Vb_%d" % kc]),
                               ["qT_%d_%d" % (h, t0)], t0, n, kcs, out_fn)
            elif kind == 1:
                for hh in range(8):
                    h = 8 * g + hh
                    kvl = hh // 4
                    qc = h // 2
                    ph = 64 * (h % 2)
                    for ti, (t0, n) in enumerate(ST):
                        if t0 >= SEQ and not need_ctx:
                            continue
                        if t0 < SEQ:
                            kcs = ctx_kcs + [kc for kc in range(4 * ti - 1, 4 * ti + 5) if 0 <= kc < 16]
                            kmask = (lambda kc, ti=ti: (kc - 4 * ti + 1) if kc < 16 else None)

                            def krange(kc, ti=ti, n=n):
                                if kc >= 16:
                                    return (0, n)
                                rel = kc - 4 * ti
                                return (128 * max(0, rel - 1), 128 * (min(3, rel + 1) + 1))
                        else:
                            kcs = ctx_kcs
                            kmask = None
                            krange = None

                        def out_fn(pO, pZ, nO, nZ, h=h, qc=qc, ph=ph, t0=t0, n=n):
                            zs, zsn = nS()
                            P.act(lambda e: e.activation(out=zs[64:128, 0:n], in_=pO[0][64:128, 0:n], func=AF.Ln, bias=esb[64:128, h:h + 1], scale=1.0),
                                  r=[nO[0], "esb"], w=[zsn])
                            P.act(lambda e: e.activation(out=zs[64:128, 0:n], in_=zs[64:128, 0:n], func=AF.Exp, scale=-1.0), r=[zsn], w=[zsn])
                            P.dve(lambda e: e.tensor_tensor(out=qT[ph:ph + 64, qc, t0:t0 + n], in0=pO[0][0:64, 0:n], in1=zs[64:128, 0:n], op=ALU.mult),
                                  r=[nO[0], zsn], w=["qT_%d_%d" % (qc, t0)])

                        if ph == 0:
                            fill_qz(qT[0:64, qc, t0:t0 + n], None, n, ["qT_%d_%d" % (qc, t0)])
                        else:
                            fill_qz(None, qT[64:128, qc, t0:t0 + n], n, ["qT_%d_%d" % (qc, t0)])
                        attend([qz[:, h % 2, 0:n]],
                               lambda m, kc, kvl=kvl: (kT[:, kvl, kc * 128:(kc + 1) * 128], names_fm("kT", kvl, kc * 128, 128)),
                               lambda kc, kvl=kvl: (Vb[:, kc, kvl * 128:(kvl + 1) * 128], ["Vb_%d" % kc]),
                               ["S4"], t0, n, kcs, out_fn, sink_h=h, kmask=kmask, krange=krange)
            else:
                for j in range(4):
                    h = 4 * g + j
                    for (t0, n) in ST:
                        if t0 >= SEQ and not need_ctx:
                            continue
                        kcs = lat_kcs if t0 < SEQ else ctx_kcs

                        def out_fn(pO, pZ, nO, nZ, h=h, t0=t0, n=n):
                            S0, S1, S2, S3_, S4_ = S
                            P.dve(lambda e: e.tensor_copy(out=S0[:, 0:n], in_=pZ[0][:, 0:n]), r=[nZ[0]], w=["S0"])
                            P.dve(lambda e: e.tensor_copy(out=S1[:, 0:n], in_=pZ[1][:, 0:n]), r=[nZ[1]], w=["S1"])
                            P.dve(lambda e: e.tensor_copy(out=S2[:, 0:n], in_=pO[0][:, 0:n]), r=[nO[0]], w=["S2"])
                            P.dve(lambda e: e.tensor_copy(out=S3_[:, 0:n], in_=pO[1][:, 0:n]), r=[nO[1]], w=["S3"])
                            P.act(lambda e: e.activation(out=S0[:, 0:n], in_=S0[:, 0:n], func=AF.Ln), r=["S0"], w=["S0"])
                            P.act(lambda e: e.activation(out=S1[:, 0:n], in_=S1[:, 0:n], func=AF.Ln), r=["S1"], w=["S1"])
                            P.act(lambda e: e.activation(out=S0[:, 0:n], in_=S0[:, 0:n], func=AF.Exp, scale=-1.0), r=["S0"], w=["S0"])
                            P.act(lambda e: e.activation(out=S1[:, 0:n], in_=S1[:, 0:n], func=AF.Exp, scale=-1.0), r=["S1"], w=["S1"])
                            P.dve(lambda e: e.tensor_tensor(out=S2[:, 0:n], in0=S2[:, 0:n], in1=S0[:, 0:n], op=ALU.mult), r=["S2", "S0"], w=["S2"])
                            P.pool(lambda e: e.tensor_tensor(out=S3_[:, 0:n], in0=S3_[:, 0:n], in1=S1[:, 0:n], op=ALU.mult), r=["S3", "S1"], w=["S3"])
                            P.dve(lambda e: e.scalar_tensor_tensor(out=qT[:, h, t0:t0 + n], in0=S3_[:, 0:n], scalar=small[:, 6:7], in1=S2[:, 0:n],
                                                                   op0=ALU.mult, op1=ALU.add), r=["S2", "S3", "small"], w=["qT_%d_%d" % (h, t0)])

                        fill_qz(qT[0:64, h, t0:t0 + n], qT[64:128, h, t0:t0 + n], n, ["qT_%d_%d" % (h, t0)])
                        attend([qz[:, 0, 0:n], qz[:, 1, 0:n]],
                               lambda m, kc, j=j: (kT[:, j, kc * 128:(kc + 1) * 128], names_fm("kT", j, kc * 128, 128)),
                               lambda kc, j=j: (Vb[:, kc, j * 128:(j + 1) * 128], ["Vb_%d" % kc]),
                               ["S4"], t0, n, kcs, out_fn)
            if kind == 2:
                kk = 0
                for j in range(4):
                    h = 4 * g + j
                    for (t0, n) in ST:
                        if t0 >= SEQ and not need_ctx:
                            continue
                        pb, pbn = ps[kk % 4], PS[kk % 4]
                        kk += 1
                        sq, sqn = nB()
                        onm = "qT_%d_%d" % (h, t0)
                        P.act(lambda e, sq=sq, h=h, t0=t0, n=n: e.activation(out=sq[:, 0:n], in_=qT[:, h, t0:t0 + n], func=AF.Square), r=[onm], w=[sqn])
                        P.pe(lambda e, sq=sq, pb=pb, n=n: e.matmul(pb[:, 0:n], lhsT=ones_bf[:], rhs=sq[:, 0:n], start=True, stop=True),
                             r=[sqn, "ones_bf"], w=[pbn])
                        rs, rsn = nS()
                        P.act(lambda e, rs=rs, pb=pb, n=n: e.activation(out=rs[:, 0:n], in_=pb[:, 0:n], func=AF.Ln, scale=1.0 / 128, bias=small[:, 1:2]),
                              r=[pbn, "small"], w=[rsn])
                        P.act(lambda e, rs=rs, n=n: e.activation(out=rs[:, 0:n], in_=rs[:, 0:n], func=AF.Exp, scale=-0.5), r=[rsn], w=[rsn])
                        P.dve(lambda e, rs=rs, h=h, t0=t0, n=n: e.scalar_tensor_tensor(out=qT[:, h, t0:t0 + n], in0=qT[:, h, t0:t0 + n], scalar=small[:, 7:8],
                                                                                       in1=rs[:, 0:n], op0=ALU.mult, op1=ALU.mult),
                              r=[onm, rsn, "small"], w=[onm])
            P.barrier()

        def _d_p2():
            for c in range(8):
                dump(qT[:, c, 0:1024], c * 128, 1024)
            for c in range(8):
                dump(qT[:, c, 2048:2304], 1024 + c * 128, 256)
        checkpoint("p2_%d" % i, _d_p2)

        load_gate(i, 2)
        load_ln(i, 0)
        P.dma("sp", lambda e: e.dma_start(out=wr[:, :, 0:4], in_=I["moe_w_group"][i].rearrange("(c p) n -> p c n", p=128)), w=["wr"])
        P.dma("sp", lambda e: e.dma_start(out=wr[:, :, 4:36], in_=I["moe_w_expert"][i].rearrange("(c p) n -> p c n", p=128)), w=["wr"])
        bcast_row(lambda c0, w_: brb[:, c0:c0 + w_], 4, I["moe_b_group"][i, :], ["brb"])
        bcast_row(lambda c0, w_: brb[:, 4 + c0:4 + c0 + w_], 32, I["moe_b_expert"][i, :], ["brb"])
        def _d_p3a():
            dump(gb[:, 0, :], 0, 1024, False)
            dump(gb[:, 1, :], 128, 1024, False)
            dump(lnp[:, 0, :], 256, 1024, False)
            dump(lnp[:, 1, :], 384, 1024, False)
            dump(brb[:], 512, 36, False)
            dump(w_o[:, 0, :], 640, 1024)
        checkpoint("p3a_%d" % i, _d_p3a)
        def p3_a(t):
            lat = t < 16
            s = t % 2
            if i == 0:
                src_d = I["x"][t * 128:(t + 1) * 128, :] if lat else I["ctx"][(t - 16) * 128:(t - 15) * 128, :]
            else:
                src_d = xs[t * 128:(t + 1) * 128, :]
            P.dma("sp", lambda e, s=s, src_d=src_d: e.dma_start(out=xt[s][:], in_=src_d), r=["xs_%d" % t], w=["xt%d" % s])
            qn = []
            for c in range(8):
                qn += names_fm("qT", c, t * 128, 128)
            for half in range(2):
                py, pyn = ps[half], PS[half]
                for c in range(8):
                    P.pe(lambda e, c=c, py=py, t=t, half=half: e.matmul(py[:], lhsT=qT[:, c, t * 128:(t + 1) * 128],
                                                                         rhs=w_o[:, c, half * 512:(half + 1) * 512],
                                                                         start=(c == 0), stop=(c == 7)),
                         r=["w_o"] + qn, w=[pyn])
                tb, tbn = S[half], "S%d" % half
                P.dve(lambda e, tb=tb, py=py, half=half, lat=lat: e.tensor_tensor(out=tb[:], in0=py[:], in1=gb[:, 0 if lat else 1, half * 512:(half + 1) * 512],
                                                                                   op=ALU.mult), r=[pyn, "gb"], w=[tbn])
                P.dve(lambda e, tb=tb, t=t, half=half, s=s: e.scalar_tensor_tensor(out=acc[:, t, half * 512:(half + 1) * 512],
                                                                                    in0=xt[s][:, half * 512:(half + 1) * 512], scalar=ALPHA,
                                                                                    in1=tb[:], op0=ALU.mult, op1=ALU.add),
                      r=[tbn, "xt%d" % s], w=["acc_%d" % t])

        def p3_ln(t):
            layer_norm_tile(acc[:, t, :], acc[:, t, :], "acc_%d" % t, "acc_%d" % t)

        def p3_b(t):
            lat = t < 16
            build_hT_tile(acc[:, t, :], "acc_%d" % t, t, 5 if lat else 7, 4 if lat else 6, True, 2 + 2 * (t % 2))
            pr, prn = ps[6 + t % 2], PS[6 + t % 2]
            for c in range(8):
                P.pe(lambda e, c=c, pr=pr: e.matmul(pr[:, 0:36], lhsT=hTf[:, c, :], rhs=wr[:, c, :], start=(c == 0), stop=(c == 7)),
                     r=["hTf_%d" % c, "wr"], w=[prn])
            P.act(lambda e, t=t: e.activation(out=acc[:, t, :], in_=acc[:, t, :], func=AF.Identity, scale=ALPHA),
                  r=["acc_%d" % t], w=["acc_%d" % t])
            L = rscr[:, 32:68]
            P.dve(lambda e, pr=pr: e.tensor_tensor(out=rscr[:, 32:68], in0=pr[:, 0:36], in1=brb[:], op=ALU.add), r=[prn, "brb"], w=["L"])
            E3 = rscr[:, 36:68].rearrange("p (g e) -> p g e", g=4)
            gmax, gsum, gsel = rscr[:, 70:71], rscr[:, 72:73], rscr[:, 76:80]
            ge, dlt = rscr[:, 80:84], rscr[:, 84:88]
            m1, m2, w1, w2 = rscr[:, 88:92], rscr[:, 92:96], rscr[:, 96:100], rscr[:, 100:104]
            eq1 = rscr[:, 104:136].rearrange("p (g e) -> p g e", g=4)
            eq2 = rscr[:, 136:168].rearrange("p (g e) -> p g e", g=4)
            E2 = rscr[:, 168:200].rearrange("p (g e) -> p g e", g=4)
            gw = rscr[:, 200:204]

            def bc(ap4):
                return ap4.unsqueeze(2).broadcast_to([128, 4, 8])

            rt_ = ["L", "rt_"]
            P.dve(lambda e: e.tensor_reduce(out=gmax, in_=rscr[:, 32:36], axis=AX.X, op=ALU.max), r=["L"], w=["rt_"])
            P.dve(lambda e: e.tensor_scalar(out=gsel, in0=rscr[:, 32:36], scalar1=gmax, scalar2=None, op0=ALU.is_equal), r=rt_, w=["rt_"])
            P.dve(lambda e: e.tensor_scalar(out=ge, in0=rscr[:, 32:36], scalar1=gmax, scalar2=None, op0=ALU.subtract), r=rt_, w=["rt_"])
            P.dve(lambda e: e.tensor_reduce(out=m1, in_=E3, axis=AX.X, op=ALU.max), r=rt_, w=["rt_"])
            P.dve(lambda e: e.tensor_tensor(out=eq1, in0=E3, in1=bc(m1), op=ALU.is_equal), r=rt_, w=["rt_"])
            P.dve(lambda e: e.scalar_tensor_tensor(out=E2, in0=eq1, scalar=-1e30, in1=E3, op0=ALU.mult, op1=ALU.add), r=rt_, w=["rt_"])
            P.dve(lambda e: e.tensor_reduce(out=m2, in_=E2, axis=AX.X, op=ALU.max), r=rt_, w=["rt_"])
            P.dve(lambda e: e.tensor_tensor(out=eq2, in0=E2, in1=bc(m2), op=ALU.is_equal), r=rt_, w=["rt_"])
            P.dve(lambda e: e.tensor_tensor(out=dlt, in0=m2, in1=m1, op=ALU.subtract), r=rt_, w=["rt_"])
            P.act(lambda e: e.activation(out=rscr[:, 80:88], in_=rscr[:, 80:88], func=AF.Exp), r=["rt_"], w=["rt_b"])
            P.dve(lambda e: e.tensor_reduce(out=gsum, in_=ge, axis=AX.X, op=ALU.add), r=["rt_b", "rt_"], w=["rt_"])
            P.dve(lambda e: e.reciprocal(out=gsum, in_=gsum), r=rt_, w=["rt_"])
            P.dve(lambda e: e.tensor_scalar(out=gw, in0=gsel, scalar1=gsum, scalar2=None, op0=ALU.mult), r=rt_, w=["rt_"])
            P.dve(lambda e: e.tensor_scalar(out=w1, in0=dlt, scalar1=1.0, scalar2=None, op0=ALU.add), r=["rt_b", "rt_"], w=["rt_"])
            P.dve(lambda e: e.reciprocal(out=w1, in_=w1), r=rt_, w=["rt_"])
            P.dve(lambda e: e.tensor_tensor(out=w2, in0=dlt, in1=w1, op=ALU.mult), r=["rt_", "rt_b"], w=["rt_"])
            P.dve(lambda e: e.tensor_tensor(out=w1, in0=w1, in1=gw, op=ALU.mult), r=rt_, w=["rt_"])
            P.dve(lambda e: e.tensor_tensor(out=w2, in0=w2, in1=gw, op=ALU.mult), r=rt_, w=["rt_"])
            P.dve(lambda e: e.tensor_tensor(out=eq1, in0=eq1, in1=bc(w1), op=ALU.mult), r=rt_, w=["rt_"])
            P.dve(lambda e: e.tensor_tensor(out=eq2, in0=eq2, in1=bc(w2), op=ALU.mult), r=rt_, w=["rt_"])
            P.dve(lambda e, t=t: e.tensor_tensor(out=comb[:, t, :].rearrange("p (g e) -> p g e", g=4), in0=eq1, in1=eq2, op=ALU.add),
                  r=rt_, w=["comb_%d" % t])

        for t in range(n_tok_tiles + 2):
            if t < n_tok_tiles:
                p3_a(t)
            if 1 <= t < n_tok_tiles + 1:
                p3_ln(t - 1)
            if t >= 2:
                p3_b(t - 2)
        P.barrier()

        def _d_p3():
            for t in range(n_tok_tiles):
                dump(acc[:, t, :], t * 128, 1024, False)
        checkpoint("p3_%d" % i, _d_p3)

        def _d_p3c():
            for t in range(n_tok_tiles):
                dump(comb[:, t, :], t * 128, 32, False)
        checkpoint("p3c_%d" % i, _d_p3c)

        load_gate(i, 5)
        load_ln(i, 1)
        nsup = 5 if need_ctx else 4
        for ex in range(n_experts):
            g_, j_ = ex // 8, ex % 8
            P.dma("pool", lambda e, g_=g_, j_=j_: e.dma_start(out=gu[:, 0, :, :], in_=I["moe_w_gate"][i, g_, j_].rearrange("(c p) n -> p c n", p=128)), w=["gu0"])
            P.dma("pool", lambda e, g_=g_, j_=j_: e.dma_start(out=gu[:, 1, :, :], in_=I["moe_w_up"][i, g_, j_].rearrange("(c p) n -> p c n", p=128)), w=["gu1"])
            P.dma("pool", lambda e, g_=g_, j_=j_: e.dma_start(out=dr[:], in_=I["moe_w_down"][i, g_, j_].rearrange("(c p) n -> p c n", p=128)), w=["dr"])
            for fc in range(4):
                P.pool(lambda e, fc=fc: e.tensor_tensor(out=dl[:, fc, :], in0=dr[:, fc, :], in1=gb[:, 0, :], op=ALU.mult), r=["dr", "gb"], w=["dl"])
            k = 0
            for fc in range(4):
                for (t0, n) in ST[:nsup]:
                    a = k % 2
                    k += 1
                    pG, pGn, pU, pUn = ps[a], PS[a], ps[2 + a], PS[2 + a]
                    for kc in range(8):
                        P.pe(lambda e, kc=kc, fc=fc, pG=pG, t0=t0, n=n: e.matmul(pG[:, 0:n], lhsT=gu[:, 0, kc, fc * 128:(fc + 1) * 128],
                                                                                  rhs=hT[:, kc, t0:t0 + n], start=(kc == 0), stop=(kc == 7)),
                             r=["gu0"] + hT_names(t0, n), w=[pGn])
                    for kc in range(8):
                        P.pe(lambda e, kc=kc, fc=fc, pU=pU, t0=t0, n=n: e.matmul(pU[:, 0:n], lhsT=gu[:, 1, kc, fc * 128:(fc + 1) * 128],
                                                                                  rhs=hT[:, kc, t0:t0 + n], start=(kc == 0), stop=(kc == 7)),
                             r=["gu1"] + hT_names(t0, n), w=[pUn])
                    hn = "hid_%d" % t0
                    P.act(lambda e, fc=fc, pG=pG, t0=t0, n=n: e.activation(out=hid[:, fc, t0:t0 + n], in_=pG[:, 0:n], func=AF.Silu), r=[pGn], w=[hn])
                    P.dve(lambda e, fc=fc, pU=pU, t0=t0, n=n: e.tensor_tensor(out=hid[:, fc, t0:t0 + n], in0=hid[:, fc, t0:t0 + n], in1=pU[:, 0:n], op=ALU.mult),
                          r=[pUn, hn], w=[hn])
            for t in range(n_tok_tiles):
                lat = t < 16
                wd = dl if lat else dr
                wdn = "dl" if lat else "dr"
                for half in range(2):
                    a = 4 + 2 * (t % 2) + half
                    py, pyn = ps[a], PS[a]
                    for fc in range(4):
                        P.pe(lambda e, fc=fc, py=py, t=t, half=half, wd=wd: e.matmul(py[:], lhsT=hid[:, fc, t * 128:(t + 1) * 128],
                                                                                      rhs=wd[:, fc, half * 512:(half + 1) * 512],
                                                                                      start=(fc == 0), stop=(fc == 3)),
                             r=[wdn, "hid_%d" % ((t // 4) * 512)], w=[pyn])
                    cw = comb[:, t, ex:ex + 1]
                    dst = acc[:, t, half * 512:(half + 1) * 512]
                    if lat:
                        P.dve(lambda e, py=py, cw=cw, dst=dst: e.scalar_tensor_tensor(out=dst, in0=py[:], scalar=cw, in1=dst, op0=ALU.mult, op1=ALU.add),
                              r=[pyn, "comb_%d" % t, "acc_%d" % t], w=["acc_%d" % t])
                    else:
                        tb, tbn = S[half], "S%d" % half
                        P.dve(lambda e, tb=tb, py=py, half=half: e.tensor_tensor(out=tb[:], in0=py[:], in1=gb[:, 1, half * 512:(half + 1) * 512], op=ALU.mult),
                              r=[pyn, "gb"], w=[tbn])
                        P.dve(lambda e, tb=tb, cw=cw, dst=dst: e.scalar_tensor_tensor(out=dst, in0=tb[:], scalar=cw, in1=dst, op0=ALU.mult, op1=ALU.add),
                              r=[tbn, "comb_%d" % t, "acc_%d" % t], w=["acc_%d" % t])
        P.barrier()

    try:
        for i_ in range(n_layers):
            layer(i_)
    except _Stop:
        with nc.allow_non_contiguous_dma(reason="dbg"):
            P.emit()
        return nc

    nfin = 16 if n_layers == DEPTH else NTILE
    for t in range(nfin):
        s = t % 2
        layer_norm_tile(acc[:, t, :], xt[s][:], "acc_%d" % t, "xt%d" % s)
        if n_layers == DEPTH or t < 16:
            P.dma("sp", lambda e, s=s, t=t: e.dma_start(out=out[t * 128:(t + 1) * 128, :], in_=xt[s][:]), r=["xt%d" % s], w=["out_%d" % t])
        if dbg:
            P.dma("sp", lambda e, s=s, t=t: e.dma_start(out=dbg_x[t * 128:(t + 1) * 128, :], in_=xt[s][:]), r=["xt%d" % s], w=["dbg_%d" % t])
    with nc.allow_non_contiguous_dma(reason="small per-partition parameter loads"):
        P.emit()
    return nc


_CONSTS = None


def kernel(**inputs):
    global _CONSTS
    if _CONSTS is None:
        _CONSTS = _consts()
    nc = build_nc()
    shared = {}
    for k, shp in INPUT_SHAPES.items():
        if k in ("x", "c", "ctx"):
            continue
        src = _CONSTS[k] if k.startswith("k_") else inputs[k]
        shared[k] = np.ascontiguousarray(np.asarray(src, dtype=np.float32)).reshape(shp)
    in_maps = []
    for b in range(8):
        m = dict(shared)
        m["x"] = np.ascontiguousarray(np.asarray(inputs["x"][b], dtype=np.float32))
        m["c"] = np.ascontiguousarray(np.asarray(inputs["c"][b], dtype=np.float32))
        m["ctx"] = np.ascontiguousarray(np.asarray(inputs["ctx"][b], dtype=np.float32))
        in_maps.append(m)
    res = run_bass_kernel_spmd(nc, in_maps, core_ids=list(range(8)))
    return np.stack([np.asarray(r["out"], dtype=np.float32) for r in res.results], axis=0)
```
